# Optimizing a Trainium2 kernel written in Bass

```python
import jax, jax.numpy as jnp
from jax import lax
import numpy as np

D_MODEL = 2048
BATCH = 4
SEQ = 2048
DEPTH = 4
DEC_BATCH = 32
DEC_SEQ = 1
PAST_LEN = 16384
PAGE_SIZE = 128

HEAD_DIM = 64
A_HEADS = 16
A_KV_HEADS = 4
A_GROUP = A_HEADS // A_KV_HEADS
WINDOW = 128
BLOCK = WINDOW
ROPE_THETA = 10000.0
A_Q = A_HEADS * HEAD_DIM
A_KV = A_KV_HEADS * HEAD_DIM
A_COLS = A_Q + 2 * A_KV
B_HEADS = 16
B_WIDTH = B_HEADS * HEAD_DIM
D_DECAY = 64
D_AAA = 64
D_GATE = 160
B_COLS = 3 * B_WIDTH + D_DECAY + D_AAA + D_GATE
GN_EPS = 64e-5
AB_COLS = A_COLS + B_COLS
MIX_WIDTH = A_Q + B_WIDTH
CONV_W = 3
D_CONV = D_MODEL
D_FF = 5632
NORM_EPS = 1e-6
N_AB = (DEPTH + 1) // 2
N_CV = DEPTH // 2

kernel_name = 'hybrid_swa_rwkv7_shortconv_macaron_step'

F32 = jnp.float32


def rmsnorm(x, g):
    x32 = x.astype(F32)
    y = x32 * lax.rsqrt(jnp.mean(x32 * x32, axis=-1, keepdims=True) + NORM_EPS)
    return (y * g.astype(F32)).astype(x.dtype)


def swiglu(x, w_gu, w_down):
    gate, up = jnp.split(x @ w_gu, 2, axis=-1)
    return (jax.nn.silu(gate) * up) @ w_down


def rope(x, pos):
    half = HEAD_DIM // 2
    freqs = ROPE_THETA ** (-jnp.arange(half, dtype=F32) / half)
    ang = pos.astype(F32)[:, None] * freqs[None, :]
    cos = jnp.cos(ang)[None, :, None, :]
    sin = jnp.sin(ang)[None, :, None, :]
    x32 = x.astype(F32)
    x1, x2 = x32[..., :half], x32[..., half:]
    return jnp.concatenate([x1 * cos - x2 * sin, x2 * cos + x1 * sin], axis=-1).astype(x.dtype)


def sink_softmax(s, sink):
    m = jnp.maximum(jnp.max(s, axis=-1, keepdims=True), sink)
    e = jnp.exp(s - m)
    return e / (jnp.sum(e, axis=-1, keepdims=True) + jnp.exp(sink - m))


def swa_banded(q, k, v, sinks):
    b, S = q.shape[:2]
    nb = S // BLOCK
    qb = q.reshape(b, nb, BLOCK, A_KV_HEADS, A_GROUP, HEAD_DIM)

    def band(t):
        tb = t.reshape(b, nb, BLOCK, A_KV_HEADS, HEAD_DIM)
        prev = jnp.concatenate([jnp.zeros_like(tb[:, :1]), tb[:, :-1]], axis=1)
        return jnp.concatenate([prev, tb], axis=2)

    kw, vw = band(k), band(v)
    s = jnp.einsum('bnqkgd,bnckd->bnkgqc', qb, kw).astype(F32) * (HEAD_DIM ** -0.5)
    qi = jnp.arange(BLOCK)[:, None]
    kc = jnp.arange(2 * BLOCK)[None, :]
    diff = BLOCK + qi - kc
    in_band = (diff >= 0) & (diff <= WINDOW)
    has_prev = jnp.arange(nb)[:, None, None] > 0
    mask = in_band[None] & (has_prev | (kc >= BLOCK)[None])
    s = jnp.where(mask[None, :, None, None], s, -jnp.inf)
    p = sink_softmax(s, sinks.astype(F32).reshape(A_KV_HEADS, A_GROUP)[None, None, :, :, None, None])
    o = jnp.einsum('bnkgqc,bnckd->bnqkgd', p.astype(vw.dtype), vw)
    return o.reshape(b, S, A_Q)


def swa_cached(q, k, v, ck, cv, sinks):
    b, T = q.shape[:2]
    kw = jnp.concatenate([ck.astype(k.dtype), k], axis=1)
    vw = jnp.concatenate([cv.astype(v.dtype), v], axis=1)
    qg = q.reshape(b, T, A_KV_HEADS, A_GROUP, HEAD_DIM)
    s = jnp.einsum('btkgd,bckd->bkgtc', qg, kw).astype(F32) * (HEAD_DIM ** -0.5)
    diff = (jnp.arange(T)[:, None] + WINDOW) - jnp.arange(WINDOW + T)[None, :]
    mask = (diff >= 0) & (diff <= WINDOW)
    s = jnp.where(mask, s, -jnp.inf)
    p = sink_softmax(s, sinks.astype(F32).reshape(A_KV_HEADS, A_GROUP)[None, :, :, None, None])
    o = jnp.einsum('bkgtc,bckd->btkgd', p.astype(vw.dtype), vw).reshape(b, T, A_Q)
    return o, kw[:, -WINDOW:], vw[:, -WINDOW:]


def rwkv_mix(pb, shift0, s0, mu, w0, w_decay, a0, w_aaa, w_gate, k_k, k_a, r_k, gn_w, gn_b):
    b, T, _ = pb.shape
    prev = jnp.concatenate([shift0[:, None].astype(pb.dtype), pb[:, :-1]], axis=1)
    xm = pb + (prev - pb) * mu
    r, k, v, dw, da, dg = jnp.split(
        xm, [B_WIDTH, 2 * B_WIDTH, 3 * B_WIDTH, 3 * B_WIDTH + D_DECAY, 3 * B_WIDTH + D_DECAY + D_AAA], axis=-1)
    w_log = -jax.nn.softplus(-(w0 + jnp.tanh(dw) @ w_decay).astype(F32)) - 0.5
    decay = jnp.exp(-jnp.exp(w_log))
    a = jax.nn.sigmoid((a0 + da @ w_aaa).astype(F32))
    g = jax.nn.sigmoid(dg) @ w_gate

    def heads(t):
        return t.astype(F32).reshape(b, T, B_HEADS, HEAD_DIM)

    def per_head(wv):
        return wv.astype(F32).reshape(B_HEADS, HEAD_DIM)

    r, k, v, decay, a = heads(r), heads(k), heads(v), heads(decay), heads(a)
    kk = k * per_head(k_k)
    kk = kk / jnp.maximum(jnp.sqrt(jnp.sum(kk * kk, axis=-1, keepdims=True)), 1e-12)
    k = k * (1.0 + (a - 1.0) * per_head(k_a))

    def step(S, inp):
        r_t, k_t, v_t, w_t, kk_t, a_t = inp
        S = (S * w_t[:, :, None, :]
             + jnp.einsum('bhvk,bhk->bhv', S, -kk_t)[..., None] * (kk_t * a_t)[:, :, None, :]
             + v_t[..., None] * k_t[:, :, None, :])
        return S, jnp.einsum('bhvk,bhk->bhv', S, r_t)

    seq = tuple(jnp.moveaxis(t, 1, 0) for t in (r, k, v, decay, kk, a))
    S, y = lax.scan(step, s0.astype(F32), seq)
    y = jnp.moveaxis(y, 0, 1)
    mean = jnp.mean(y, axis=-1, keepdims=True)
    var = jnp.mean((y - mean) ** 2, axis=-1, keepdims=True)
    y = (y - mean) * lax.rsqrt(var + GN_EPS) * per_head(gn_w) + per_head(gn_b)
    y = y + jnp.sum(r * k * per_head(r_k), axis=-1, keepdims=True) * v
    out = y.reshape(b, T, B_WIDTH).astype(pb.dtype) * g
    return out, S, pb[:, -1]


def ab_mixer(h, pos, swa_cache, s0, shift0, p, i):
    b, T, _ = h.shape
    proj = h @ p['ab_w_in'][i]
    q, k, v, pb = jnp.split(proj, [A_Q, A_Q + A_KV, A_COLS], axis=-1)
    q = rope(q.reshape(b, T, A_HEADS, HEAD_DIM), pos)
    k = rope(k.reshape(b, T, A_KV_HEADS, HEAD_DIM), pos)
    v = v.reshape(b, T, A_KV_HEADS, HEAD_DIM)
    sinks = p['attn_sinks'][i]
    if swa_cache is None:
        a_out = swa_banded(q, k, v, sinks)
        nk, nv = k[:, -WINDOW:], v[:, -WINDOW:]
    else:
        a_out, nk, nv = swa_cached(q, k, v, swa_cache[0], swa_cache[1], sinks)
    b_out, ns, nsh = rwkv_mix(pb, shift0, s0, p['rwkv_mu'][i], p['rwkv_w0'][i], p['rwkv_w_decay'][i],
                              p['rwkv_a0'][i], p['rwkv_w_aaa'][i], p['rwkv_w_gate'][i], p['rwkv_k_k'][i],
                              p['rwkv_k_a'][i], p['rwkv_r_k'][i], p['rwkv_gn_w'][i], p['rwkv_gn_b'][i])
    out = jnp.concatenate([a_out, b_out], axis=-1) @ p['ab_w_out'][i]
    return out, nk, nv, ns.astype(h.dtype), nsh


def conv_mixer(h, buf, w_in, w_conv, w_out):
    bg, cg, hv = jnp.split(h @ w_in, 3, axis=-1)
    u = cg * hv
    T = u.shape[1]
    full = jnp.concatenate([buf.astype(u.dtype), u], axis=1)
    y = sum(full[:, j:j + T] * w_conv[j] for j in range(CONV_W))
    return (bg * y) @ w_out, full[:, -(CONV_W - 1):]


def _trunk(x, past, p):
    b, T, _ = x.shape
    start = 0 if past is None else PAST_LEN
    pos = start + jnp.arange(T, dtype=jnp.int32)
    out_k, out_v, out_s, out_sh, out_c = [], [], [], [], []
    for l in range(DEPTH):
        g = p['norm_g'][l]
        x = x + 0.5 * rmsnorm(swiglu(rmsnorm(x, g[0]), p['ffn_w_gu'][l, 0], p['ffn_w_down'][l, 0]), g[1])
        h = rmsnorm(x, g[2])
        i = l // 2
        if l % 2 == 0:
            if past is None:
                swa = None
                s0 = jnp.zeros((b, B_HEADS, HEAD_DIM, HEAD_DIM), F32)
                sh0 = jnp.zeros((b, B_COLS), x.dtype)
            else:
                swa = (past[0][i], past[1][i])
                s0, sh0 = past[2][i], past[3][i]
            m, nk, nv, ns, nsh = ab_mixer(h, pos, swa, s0, sh0, p, i)
            out_k.append(nk)
            out_v.append(nv)
            out_s.append(ns)
            out_sh.append(nsh)
        else:
            buf = jnp.zeros((b, CONV_W - 1, D_CONV), x.dtype) if past is None else past[4][i]
            m, nc = conv_mixer(h, buf, p['conv_w_in'][i], p['conv_w'][i], p['conv_w_out'][i])
            out_c.append(nc)
        x = x + rmsnorm(m, g[3])
        x = x + 0.5 * rmsnorm(swiglu(rmsnorm(x, g[4]), p['ffn_w_gu'][l, 1], p['ffn_w_down'][l, 1]), g[5])
    return x, (jnp.stack(out_k), jnp.stack(out_v), jnp.stack(out_s), jnp.stack(out_sh), jnp.stack(out_c))


def setup_inputs(seed: int = 0) -> dict:
    key = jax.random.key(seed)
    ks = iter(jax.random.split(key, 32))

    def nrm(shape, scale):
        return jax.random.normal(next(ks), shape, F32) * scale

    return {
        'x_prompt': nrm((BATCH, SEQ, D_MODEL), 1.0),
        'x_sample': nrm((DEC_BATCH, DEC_SEQ, D_MODEL), 1.0),
        'cache_swa_k': nrm((N_AB, DEC_BATCH, WINDOW, A_KV_HEADS, HEAD_DIM), 1.0),
        'cache_swa_v': nrm((N_AB, DEC_BATCH, WINDOW, A_KV_HEADS, HEAD_DIM), 1.0),
        'state_rwkv': nrm((N_AB, DEC_BATCH, B_HEADS, HEAD_DIM, HEAD_DIM), 0.5),
        'state_rwkv_shift': nrm((N_AB, DEC_BATCH, B_COLS), 1.0),
        'state_conv': nrm((N_CV, DEC_BATCH, CONV_W - 1, D_CONV), 1.0),
        'norm_g': 1.0 + nrm((DEPTH, 6, D_MODEL), 0.05),
        'ffn_w_gu': nrm((DEPTH, 2, D_MODEL, 2 * D_FF), D_MODEL ** -0.5),
        'ffn_w_down': nrm((DEPTH, 2, D_FF, D_MODEL), D_FF ** -0.5),
        'ab_w_in': nrm((N_AB, D_MODEL, AB_COLS), D_MODEL ** -0.5),
        'ab_w_out': nrm((N_AB, MIX_WIDTH, D_MODEL), MIX_WIDTH ** -0.5),
        'attn_sinks': nrm((N_AB, A_HEADS), 1.0),
        'rwkv_mu': jax.random.uniform(next(ks), (N_AB, B_COLS), F32),
        'rwkv_w0': nrm((N_AB, B_WIDTH), 1.0) - 1.0,
        'rwkv_w_decay': nrm((N_AB, D_DECAY, B_WIDTH), 0.1),
        'rwkv_a0': nrm((N_AB, B_WIDTH), 0.5),
        'rwkv_w_aaa': nrm((N_AB, D_AAA, B_WIDTH), 0.5 * D_AAA ** -0.5),
        'rwkv_w_gate': nrm((N_AB, D_GATE, B_WIDTH), D_GATE ** -0.5),
        'rwkv_k_k': 0.85 + nrm((N_AB, B_WIDTH), 0.05),
        'rwkv_k_a': 1.0 + nrm((N_AB, B_WIDTH), 0.05),
        'rwkv_r_k': nrm((N_AB, B_WIDTH), 0.1),
        'rwkv_gn_w': 1.0 + nrm((N_AB, B_WIDTH), 0.05),
        'rwkv_gn_b': nrm((N_AB, B_WIDTH), 0.01),
        'conv_w_in': nrm((N_CV, D_MODEL, 3 * D_CONV), D_MODEL ** -0.5),
        'conv_w': nrm((N_CV, CONV_W, D_CONV), CONV_W ** -0.5),
        'conv_w_out': nrm((N_CV, D_CONV, D_MODEL), D_CONV ** -0.5),
    }


def reference(x_prompt, x_sample, cache_swa_k, cache_swa_v, state_rwkv, state_rwkv_shift, state_conv,
              norm_g, ffn_w_gu, ffn_w_down, ab_w_in, ab_w_out, attn_sinks,
              rwkv_mu, rwkv_w0, rwkv_w_decay, rwkv_a0, rwkv_w_aaa, rwkv_w_gate,
              rwkv_k_k, rwkv_k_a, rwkv_r_k, rwkv_gn_w, rwkv_gn_b,
              conv_w_in, conv_w, conv_w_out):
    p = {
        'norm_g': norm_g, 'ffn_w_gu': ffn_w_gu, 'ffn_w_down': ffn_w_down,
        'ab_w_in': ab_w_in, 'ab_w_out': ab_w_out, 'attn_sinks': attn_sinks,
        'rwkv_mu': rwkv_mu, 'rwkv_w0': rwkv_w0, 'rwkv_w_decay': rwkv_w_decay, 'rwkv_a0': rwkv_a0,
        'rwkv_w_aaa': rwkv_w_aaa, 'rwkv_w_gate': rwkv_w_gate, 'rwkv_k_k': rwkv_k_k, 'rwkv_k_a': rwkv_k_a,
        'rwkv_r_k': rwkv_r_k, 'rwkv_gn_w': rwkv_gn_w, 'rwkv_gn_b': rwkv_gn_b,
        'conv_w_in': conv_w_in, 'conv_w': conv_w, 'conv_w_out': conv_w_out,
    }
    y_prompt, (p_swa_k, p_swa_v, p_rwkv, p_shift, p_conv) = _trunk(x_prompt, None, p)
    y_sample, (s_swa_k, s_swa_v, s_rwkv, s_shift, s_conv) = _trunk(
        x_sample, (cache_swa_k, cache_swa_v, state_rwkv, state_rwkv_shift, state_conv), p)
    return (y_prompt, y_sample, p_swa_k, p_swa_v, p_rwkv, p_shift, p_conv,
            s_swa_k, s_swa_v, s_rwkv, s_shift, s_conv)
```

```python
import contextlib
import numpy as np
import concourse.bass as bass
import concourse.mybir as mybir
from concourse.bass_utils import run_bass_kernel_spmd

F32 = mybir.dt.float32
BF16 = mybir.dt.bfloat16
AF = mybir.ActivationFunctionType
ALU = mybir.AluOpType
AX = mybir.AxisListType

D = 2048
DFF = 5632
NKC = 16
HD = 64
A_Q = 1024
A_KV = 256
A_COLS = 1536
B_WIDTH = 1024
B_COLS = 3360
AB_COLS = 4896
WINDOW = 128
NORM_EPS = 1e-6
GN_EPS = 64e-5
SB_BASE = 16512
SB_END = 229344

ENGS = ("pe", "act", "dve", "pool", "sp")
EPOCH = 16000
DMA_LIMIT = 4


class Op:
    __slots__ = ("eng", "fn", "deps", "idx", "is_dma", "sem", "semval", "signal", "group", "cum", "fdeps")

    def __init__(self, eng, fn, is_dma):
        self.eng = eng
        self.fn = fn
        self.deps = set()
        self.is_dma = is_dma
        self.sem = None
        self.semval = 0
        self.signal = False
        self.group = False
        self.fdeps = ()


class Prog:
    def __init__(self, nc, same_engine_sync=True):
        self.nc = nc
        self.ops = []
        self.last_w = {}
        self.readers = {}
        self.same_engine_sync = same_engine_sync
        self.dma_cnt = {}
        self.last_pe_fp32 = None

    def op(self, eng, fn, reads=(), writes=(), dma_key=None, group=False, pe_fp32=False):
        o = Op(eng, fn, dma_key is not None)
        o.idx = len(self.ops)
        deps = o.deps
        if eng == "pe":
            if pe_fp32:
                self.last_pe_fp32 = o.idx
            elif self.last_pe_fp32 is not None:
                o.fdeps = (self.last_pe_fp32,)
                deps.add(self.last_pe_fp32)
                self.last_pe_fp32 = None
        lw = self.last_w
        rd = self.readers
        for k in reads:
            w = lw.get(k)
            if w is not None:
                deps.add(w)
        for k in writes:
            w = lw.get(k)
            if w is not None:
                deps.add(w)
            r = rd.get(k)
            if r:
                deps.update(r)
        for k in reads:
            rd.setdefault(k, []).append(o.idx)
        for k in writes:
            lw[k] = o.idx
            rd[k] = []
        if dma_key is not None:
            o.sem = ("dma", dma_key)
            c = self.dma_cnt.get(dma_key, 0) + 16
            self.dma_cnt[dma_key] = c
            o.semval = c
            o.cum = c
            o.group = group
        self.ops.append(o)
        return o

    def emit(self, final_wait_eng="sp"):
        nc = self.nc
        ops = self.ops
        ses = self.same_engine_sync
        for o in ops:
            for d in o.deps:
                od = ops[d]
                if od.is_dma:
                    continue
                if od.eng == o.eng and (not ses or (ses == 2 and o.eng == "pe")) and o.eng != "pool" and d not in o.fdeps:
                    continue
                od.signal = True
        cnt = {e: 0 for e in ENGS}
        for o in ops:
            if not o.is_dma and o.signal:
                c = cnt[o.eng]
                cnt[o.eng] = c + 1
                o.sem = ("eng", o.eng, c // EPOCH)
                o.semval = c % EPOCH + 1
            elif o.is_dma and o.group:
                o.semval = self.dma_cnt[o.sem[1]]
        sem_names = sorted({o.sem for o in ops if o.sem is not None}, key=str)
        print("NSEMS", len(sem_names), "NOPS", len(ops), "SIG", cnt, "DMAMAX", max(self.dma_cnt.values()), flush=True)
        with contextlib.ExitStack() as es:
            sems = {}
            for i, sn in enumerate(sem_names):
                sems[sn] = es.enter_context(nc.semaphore("s%d" % i))
            block = es.enter_context(nc.Block())
            per_eng = {e: [o for o in ops if o.eng == e] for e in ENGS}
            final_vals = {("dma", k): v for k, v in self.dma_cnt.items()}

            def make(e):
                def body(eng):
                    waited = {}
                    inflight = []
                    for o in per_eng[e]:
                        if o.is_dma:
                            while len(inflight) >= DMA_LIMIT:
                                sn0, v0 = inflight.pop(0)
                                if waited.get(sn0, 0) < v0:
                                    eng.wait_ge(sems[sn0], v0)
                                    waited[sn0] = v0
                        need = {}
                        for d in o.deps:
                            od = ops[d]
                            if od.sem is None:
                                continue
                            if (not od.is_dma) and od.eng == e and (not ses or (ses == 2 and e == "pe")) and e != "pool" and d not in o.fdeps:
                                continue
                            if od.semval > need.get(od.sem, 0):
                                need[od.sem] = od.semval
                        for sn, v in need.items():
                            if waited.get(sn, 0) >= v:
                                continue
                            eng.wait_ge(sems[sn], v)
                            waited[sn] = v
                        ins = o.fn(eng)
                        if o.is_dma:
                            ins.then_inc(sems[o.sem], 16)
                            inflight.append((o.sem, o.cum))
                        elif o.signal:
                            ins.then_inc(sems[o.sem], 1)
                    if e == final_wait_eng:
                        for sn, v in final_vals.items():
                            if waited.get(sn, 0) < v:
                                eng.wait_ge(sems[sn], v)
                return body

            block.tensor(make("pe"))
            block.scalar(make("act"))
            block.vector(make("dve"))
            block.gpsimd(make("pool"))
            block.sync(make("sp"))


class Cfg:
    def __init__(self, TP=1024, NS=4, NPASS=2, DEPTH=4, mixers=3):
        self.TP = TP
        self.NS = NS
        self.NPASS = NPASS
        self.DEPTH = DEPTH
        self.N_AB = (DEPTH + 1) // 2
        self.N_CV = DEPTH // 2
        self.NT = TP + NS
        self.mixers = mixers
        cgs = []
        c = 0
        while c < TP:
            n = min(512, TP - c)
            cgs.append((c, n))
            c += n
        self.big = cgs
        self.small = (TP, NS)


class Builder:
    def __init__(self, cfg):
        self.cfg = cfg
        self.nc = bass.Bass("TRN2", target_bir_lowering=False)
        import os
        self.P = Prog(self.nc, same_engine_sync=int(os.environ.get('SES', '2')))
        self.off = SB_BASE
        self.unit = 0
        self.wslot = 0
        self.din = {}
        self.dout = {}

    def sb_at(self, name, shape, dt, off):
        return self.nc.alloc_sbuf_tensor_at(name, list(shape), dt, offset=SB_BASE + off)

    def dram_in(self, name, shape, dt=F32):
        t = self.nc.dram_tensor(name, list(shape), dt, kind="ExternalInput").ap()
        self.din[name] = t
        return t

    def dram_out(self, name, shape, dt=F32):
        t = self.nc.dram_tensor(name, list(shape), dt, kind="ExternalOutput").ap()
        self.dout[name] = t
        return t

    def dram_tmp(self, name, shape, dt=F32):
        return self.nc.dram_tensor(name, list(shape), dt, kind="Internal").ap()

    def mm(self, out, lhsT, rhs, start, stop, reads, writes, **kw):
        self.P.op("pe", lambda e: e.matmul(out, lhsT=lhsT, rhs=rhs, start=start, stop=stop, **kw), reads, writes,
                  pe_fp32=(lhsT.dtype == F32))

    def tr(self, out, in_, ident, reads, writes):
        self.P.op("pe", lambda e: e.transpose(out, in_, ident), reads, writes, pe_fp32=(in_.dtype == F32))

    def act(self, out, in_, func, reads, writes, bias=None, scale=None, accum_out=None):
        kw = {}
        if bias is not None:
            kw["bias"] = bias
        if scale is not None:
            kw["scale"] = scale
        if accum_out is not None:
            kw["accum_out"] = accum_out
        self.P.op("act", lambda e: e.activation(out=out, in_=in_, func=func, **kw), reads, writes)

    def tt(self, eng, out, in0, in1, op, reads, writes):
        self.P.op(eng, lambda e: e.tensor_tensor(out=out, in0=in0, in1=in1, op=op), reads, writes)

    def ts(self, eng, out, in0, s1, s2, op0, op1, reads, writes):
        if op1 is None:
            self.P.op(eng, lambda e: e.tensor_scalar(out=out, in0=in0, scalar1=s1, scalar2=None, op0=op0), reads, writes)
        else:
            self.P.op(eng, lambda e: e.tensor_scalar(out=out, in0=in0, scalar1=s1, scalar2=s2, op0=op0, op1=op1), reads, writes)

    def stt(self, eng, out, in0, scalar, in1, op0, op1, reads, writes):
        self.P.op(eng, lambda e: e.scalar_tensor_tensor(out=out, in0=in0, scalar=scalar, in1=in1, op0=op0, op1=op1), reads, writes)

    def cp(self, eng, out, in_, reads, writes):
        if eng == "act":
            self.P.op("act", lambda e: e.activation(out=out, in_=in_, func=AF.Identity), reads, writes)
        else:
            self.P.op(eng, lambda e: e.tensor_copy(out=out, in_=in_), reads, writes)

    def recip(self, out, in_, reads, writes):
        self.P.op("dve", lambda e: e.reciprocal(out=out, in_=in_), reads, writes)

    def memset(self, eng, ap, val, writes):
        self.P.op(eng, lambda e: e.memset(ap, val), (), writes)

    def dma(self, eng, out, in_, reads, writes, key, group=False, **kw):
        self.P.op(eng, lambda e: e.dma_start(out=out, in_=in_, **kw), reads, writes, dma_key=key, group=group)

    def fence(self, old, new):
        f = self.FENCE
        self.P.op("dve", lambda e: e.memset(f[:, 0:1], 0.0), [], list(old) + list(new) + ["FENCE"])

    def load_fm(self, dst, src_rows, R, ncols, keys_w):
        stg = self.sb_at("PSTG%d" % self.unit, [R, ncols], F32, self.Y_off)
        self.unit += 1
        self.dma("sp", stg[:], src_rows, [], ["YR"], "fmload")
        nch = (ncols + 127) // 128
        for c in range(nch):
            n = min(128, ncols - c * 128)
            ps = self.PS[c % 2]
            self.tr(ps[0:n, 0:R], stg[:, c * 128:c * 128 + n], self.IDENT[0:R, 0:R], ["YR", "IDENT"], ["PS%d" % (c % 2)])
            self.cp("dve", dst[0:n, :, c], ps[0:n, 0:R], ["PS%d" % (c % 2)], keys_w)

    def build(self):
        cfg = self.cfg
        nc = self.nc
        NT, TP, NS = cfg.NT, cfg.TP, cfg.NS
        L = cfg.DEPTH
        NAB, NCV = cfg.N_AB, cfg.N_CV
        SPC = cfg.NPASS * NS
        self.SPC = SPC
        xp = self.dram_in("xp", [cfg.NPASS * TP, D])
        xs = self.dram_in("xs", [SPC, D])
        self.norm_g = self.dram_in("norm_g", [L, 6, D])
        self.w_gu = self.dram_in("ffn_w_gu", [L, 2, D, 2 * DFF])
        self.w_dn = self.dram_in("ffn_w_down", [L, 2, DFF, D])
        yp = self.dram_out("y_prompt", [cfg.NPASS * TP, D])
        ys = self.dram_out("y_sample", [SPC, D])
        if cfg.mixers:
            self.cv_w_in = self.dram_in("conv_w_in", [NCV, D, 3 * D])
            self.cv_w = self.dram_in("conv_w", [NCV, 3, D])
            self.cv_w_out = self.dram_in("conv_w_out", [NCV, D, D])
            self.st_conv = self.dram_in("state_conv", [NCV, SPC, 2, D])
            self.p_conv = self.dram_out("p_conv", [NCV, 2, D])
            self.s_conv = self.dram_out("s_conv", [NCV, SPC, 2, D])
            self.CONVC = self.dram_tmp("CONVC", [NCV, 128, NKC, 2])
            self.ab_w_in = self.dram_in("ab_w_in", [NAB, D, AB_COLS])
            self.ab_w_out = self.dram_in("ab_w_out", [NAB, 2048, D])
            self.sinks = self.dram_in("attn_sinks", [NAB, 16])
            self.rope_t = self.dram_in("rope_t", [cfg.NPASS, 2, 128, NT])
            self.masks = self.dram_in("swa_masks", [2, 128, 256])
            self.cache_k = self.dram_in("cache_swa_k", [NAB, SPC, 128, 256])
            self.cache_v = self.dram_in("cache_swa_v", [NAB, SPC, 128, 256])
            self.p_swa_k = self.dram_out("p_swa_k", [NAB, 128, 256])
            self.p_swa_v = self.dram_out("p_swa_v", [NAB, 128, 256])
            self.s_swa_k = self.dram_out("s_swa_k", [NAB, SPC, 128, 256])
            self.s_swa_v = self.dram_out("s_swa_v", [NAB, SPC, 128, 256])
            self.KCAR = self.dram_tmp("KCAR", [NAB, 128, 4, 128], BF16)
            self.VCAR = self.dram_tmp("VCAR", [NAB, 128, 256], BF16)
            self.PBD = self.dram_tmp("PBD", [27, 128, 1 + NT])
            self.rw_par = self.dram_in("rwkv_par", [NAB, 7, 1024])
            self.rw_mu = self.dram_in("rwkv_mu", [NAB, B_COLS])
            self.rw_wdec = self.dram_in("rwkv_w_decay", [NAB, 64, 1024])
            self.rw_waaa = self.dram_in("rwkv_w_aaa", [NAB, 64, 1024])
            self.rw_wgate = self.dram_in("rwkv_w_gate", [NAB, 160, 1024])
            self.st_rwkv = self.dram_in("state_rwkv", [NAB, SPC, 16, 64, 64])
            self.st_shift = self.dram_in("state_rwkv_shift", [NAB, SPC, B_COLS])
            self.p_rwkv = self.dram_out("p_rwkv", [NAB, 16, 64, 64])
            self.p_shift = self.dram_out("p_shift", [NAB, B_COLS])
            self.s_rwkv = self.dram_out("s_rwkv", [NAB, SPC, 16, 64, 64])
            self.s_shift = self.dram_out("s_shift", [NAB, SPC, B_COLS])
            self.RWQ = self.dram_tmp("RWQ", [7, 128, 8, NT])
            self.YSD = self.dram_tmp("YSD", [128, 8, NT])
            self.ZCAR = self.dram_tmp("ZCAR", [NAB, 128, 8, 64])
            self.SHCAR = self.dram_tmp("SHCAR", [NAB, 128, 27])

        o = 0

        def take(name, shape, dt, nbytes):
            nonlocal o
            o = (o + 31) // 32 * 32
            t = self.sb_at(name, shape, dt, o)
            off = o
            o += nbytes
            return t, off
        self.X, _ = take("X", [128, NKC, NT], F32, NKC * NT * 4)
        self.Y, self.Y_off = take("Y", [128, NKC, NT], F32, NKC * NT * 4)
        self.H, self.H_off = take("H", [128, NKC, NT], BF16, NKC * NT * 2)
        self.WG, self.WG_off = take("WG", [128, 4, 2048], BF16, 4 * 2048 * 2)
        self.WD, self.WD_off = take("WD", [128, 2, 2048], BF16, 2 * 2048 * 2)
        self.ACTB, self.ACTB_off = take("ACTB", [128, 2, NT], BF16, 2 * NT * 2)
        self.TMP, self.TMP_off = take("TMP", [128, 2, NT + 4], F32, 2 * (NT + 4) * 4)
        self.G, _ = take("G", [128, L * 6, NKC], F32, L * 6 * NKC * 4)
        self.GH, _ = take("GH", [128, L * 6, NKC], F32, L * 6 * NKC * 4)
        self.IDENT, _ = take("IDENT", [128, 128], F32, 512)
        self.ONESB, _ = take("ONESB", [128, 128], BF16, 256)
        self.BONES, _ = take("BONES", [128, 128], F32, 512)
        self.FENCE, _ = take("FENCE", [128, 8], F32, 32)
        self.CW, _ = take("CW", [128, max(1, NCV) * 3, NKC], F32, max(1, NCV) * 3 * NKC * 4)
        self.SCB, _ = take("SCB", [128, NS * 2, NKC], F32, NS * 2 * NKC * 4)
        self.SOUT, _ = take("SOUT", [128, NS * 2, NKC], F32, NS * 2 * NKC * 4)
        self.PAR, _ = take("PAR", [128, max(1, NAB) * 7, 8], F32, max(1, NAB) * 7 * 8 * 4)
        self.MU, _ = take("MU", [128, max(1, NAB), 27], F32, max(1, NAB) * 27 * 4)
        self.OMU, _ = take("OMU", [128, max(1, NAB), 27], F32, max(1, NAB) * 27 * 4)
        self.SINK, _ = take("SINK", [128, max(1, NAB) * 16], F32, max(1, NAB) * 16 * 4)
        self.STAT, _ = take("STAT", [128, 32], F32, 128)
        self.SHS, _ = take("SHS", [128, NS, 27], F32, NS * 27 * 4)
        self.SHP, _ = take("SHP", [128, 1, 27], F32, 108)
        o = (o + 31) // 32 * 32
        self.const_end = o
        assert SB_BASE + o <= SB_END, o
        self.RSTD = self.TMP[:, 0, 0:NT]
        self.STG = self.sb_at("STG", [128, 2, D], F32, self.Y_off)

        self.PP = [nc.alloc_psum_tensor("pp%d" % i, [128, 1024], F32) for i in range(3)]
        self.PS = [nc.alloc_psum_tensor("psm%d" % i, [128, 512], F32) for i in range(2)]

        self.memset("pool", self.IDENT[:], 1.0, ["IDENT"])
        self.P.op("pool", lambda e: e.affine_select(out=self.IDENT[:], in_=self.IDENT[:], pattern=[[-1, 128]],
                                                     compare_op=ALU.is_equal, fill=0.0, base=0, channel_multiplier=1),
                  ["IDENT"], ["IDENT"])
        self.memset("pool", self.ONESB[:], 1.0, ["ONESB"])
        self.memset("pool", self.BONES[:], 0.0, ["BONES"])
        self.memset("pool", self.BONES[0:64, 0:64], 1.0, ["BONES"])
        self.memset("pool", self.BONES[64:128, 64:128], 1.0, ["BONES"])
        self.memset("dve", self.FENCE[:], 0.0, ["FENCE"])
        self.load_fm(self.G, self.norm_g.rearrange("l k d -> (l k) d"), L * 6, D, ["G"])
        self.ts("dve", self.GH[:], self.G[:], 0.5, None, ALU.mult, None, ["G"], ["GH"])
        if cfg.mixers:
            if NCV:
                self.load_fm(self.CW, self.cv_w.rearrange("i j d -> (i j) d"), NCV * 3, D, ["CW"])
            self.load_fm(self.PAR, self.rw_par.rearrange("i k d -> (i k) d"), NAB * 7, 1024, ["PAR"])
            self.memset("dve", self.MU[:], 0.0, ["MU"])
            self.load_fm(self.MU, self.rw_mu, NAB, B_COLS, ["MU"])
            self.ts("dve", self.OMU[:], self.MU[:], -1.0, 1.0, ALU.mult, ALU.add, ["MU"], ["OMU"])
            self.dma("sp", self.SINK[:], self.sinks.rearrange("i h -> (i h)").partition_broadcast(128), [], ["SINK"], "sinkld")
            zt = self.sb_at("ZT", [128, 1024], F32, self.Y_off + 8192)
            self.memset("dve", zt[:], 0.0, ["YR2"])
            for i in range(NCV):
                self.dma("sp", self.CONVC[i].rearrange("p c j -> p (c j)"), zt[:, 0:NKC * 2], ["YR2"], ["CONVC%d" % i], "zinit", group=True)
            for i in range(NAB):
                self.dma("sp", self.ZCAR[i].rearrange("p a b -> p (a b)"), zt[:, 0:512], ["YR2"], ["ZCAR%d" % i], "zinit", group=True)
                self.dma("sp", self.SHCAR[i], zt[:, 0:27], ["YR2"], ["SHCAR%d" % i], "zinit", group=True)
            zb = self.sb_at("ZB16", [128, 512], BF16, self.Y_off + 12288)
            self.memset("dve", zb[:], 0.0, ["YR3"])
            for i in range(NAB):
                self.dma("sp", self.KCAR[i].rearrange("p a b -> p (a b)"), zb[:, :], ["YR3"], ["KCAR%d" % i], "zinit", group=True)
                self.dma("sp", self.VCAR[i], zb[:, 0:256], ["YR3"], ["VCAR%d" % i], "zinit", group=True)
            self.fence(["YR", "YR2", "YR3"], ["Y%d" % c for c in range(NKC)] + ["STG0", "STG1"])
        else:
            self.fence(["YR"], ["Y%d" % c for c in range(NKC)] + ["STG0", "STG1"])

        for ps_i in range(cfg.NPASS):
            self.load_x(xp, xs, ps_i)
            for l in range(L):
                self.ffn(l, 0)
                if cfg.mixers:
                    if l % 2 == 1:
                        import os
                        if not os.environ.get("SKIPCONV"):
                            self.conv_mixer(l, ps_i)
                    elif cfg.mixers >= 2:
                        self.ab_mixer(l, ps_i)
                self.ffn(l, 1)
            self.store_x(yp, ys, ps_i)
        self.P.emit()
        return nc

    def kx(self):
        return ["X%d" % c for c in range(NKC)]

    def ky(self):
        return ["Y%d" % c for c in range(NKC)]

    def load_x(self, xp, xs, ps_i):
        cfg = self.cfg
        TP, NS = cfg.TP, cfg.NS
        nblk = TP // 128
        yk = ["Y0", "Y1", "Y2", "Y3"]
        for tb in range(nblk + 1):
            s = tb % 2
            if tb < nblk:
                n = 128
                src = xp[ps_i * TP + tb * 128: ps_i * TP + (tb + 1) * 128, :]
                c0 = tb * 128
            else:
                n = NS
                src = xs[ps_i * NS:(ps_i + 1) * NS, :]
                c0 = TP
            self.dma("sp", self.STG[0:n, s, :], src, [], yk + ["STG%d" % s], "stg%d" % s)
            for c in range(NKC):
                pb = self.PS[c % 2]
                self.tr(pb[:, 0:n], self.STG[0:n, s, c * 128:(c + 1) * 128], self.IDENT[0:n, 0:n],
                        ["STG%d" % s, "IDENT"] + yk, ["PS%d" % (c % 2)])
                self.cp("dve" if c % 2 == 0 else "act", self.X[:, c, c0:c0 + n], pb[:, 0:n], ["PS%d" % (c % 2)], ["X%d" % c])

    def store_x(self, yp, ys, ps_i):
        cfg = self.cfg
        TP, NS = cfg.TP, cfg.NS
        nblk = TP // 128
        yk = ["Y0", "Y1", "Y2", "Y3"]
        for tb in range(nblk + 1):
            s = tb % 2
            if tb < nblk:
                n = 128
                dst = yp[ps_i * TP + tb * 128: ps_i * TP + (tb + 1) * 128, :]
                c0 = tb * 128
            else:
                n = NS
                dst = ys[ps_i * NS:(ps_i + 1) * NS, :]
                c0 = TP
            for c in range(NKC):
                pb = self.PS[c % 2]
                self.tr(pb[0:n, 0:128], self.X[:, c, c0:c0 + n], self.IDENT[:, :], ["X%d" % c, "IDENT"], ["PS%d" % (c % 2)])
                self.cp("dve" if c % 2 == 0 else "act", self.STG[0:n, s, c * 128:(c + 1) * 128], pb[0:n, 0:128],
                        ["PS%d" % (c % 2)], yk + ["STG%d" % s])
            self.dma("sp", dst, self.STG[0:n, s, :], ["STG%d" % s] + yk, [], "stg%d" % s)

    def rstd_of(self, src, src_keys):
        cfg = self.cfg
        NT = cfg.NT
        groups = cfg.big + [cfg.small]
        for c in range(NKC):
            sq = self.ACTB[:, c % 2, :]
            self.act(sq, src[:, c, :], AF.Square, [src_keys[c]], ["ACTB%d" % (c % 2)])
            for gi, (c0, n) in enumerate(groups):
                if gi < len(cfg.big):
                    out = self.PP[0][:, gi * 512:gi * 512 + n]
                    wk = "PP0"
                else:
                    out = self.PS[0][:, 0:n]
                    wk = "PS0"
                self.mm(out, self.ONESB[:, :], sq[:, c0:c0 + n], c == 0, c == NKC - 1, ["ONESB", "ACTB%d" % (c % 2)], [wk])
        for gi, (c0, n) in enumerate(groups):
            if gi < len(cfg.big):
                src_ps = self.PP[0][:, gi * 512:gi * 512 + n]
                rk = "PP0"
            else:
                src_ps = self.PS[0][:, 0:n]
                rk = "PS0"
            self.act(self.RSTD[:, c0:c0 + n], src_ps, AF.Sqrt, [rk], ["TMP0"], bias=NORM_EPS, scale=1.0 / D)
        self.recip(self.RSTD, self.RSTD, ["TMP0"], ["TMP0"])

    def prenorm(self, gidx):
        self.rstd_of(self.X, self.kx())
        for c in range(NKC):
            self.stt("dve", self.H[:, c, :], self.X[:, c, :], self.G[:, gidx, c:c + 1], self.RSTD, ALU.mult, ALU.mult,
                     ["X%d" % c, "G", "TMP0"], ["H%d" % c])

    def postnorm_add(self, gtile, gidx):
        self.rstd_of(self.Y, self.ky())
        for c in range(NKC):
            self.tt("dve", self.Y[:, c, :], self.Y[:, c, :], self.RSTD, ALU.mult, ["Y%d" % c, "TMP0"], ["Y%d" % c])
            self.stt("dve", self.X[:, c, :], self.Y[:, c, :], gtile[:, gidx, c:c + 1], self.X[:, c, :], ALU.mult, ALU.add,
                     ["Y%d" % c, "X%d" % c, "GH", "G"], ["X%d" % c])

    def next_unit(self):
        u = self.unit
        self.unit += 1
        return u

    def load_wtile(self, src_ap, ncol=128):
        slot = self.wslot % 4
        self.wslot += 1
        wt = self.WG[:, slot, :].rearrange("p (kc n) -> p kc n", n=128)
        self.dma("pool", wt[:, :, 0:ncol], src_ap, [], ["WG%d" % slot], "wg%d" % slot)
        return wt, "WG%d" % slot

    def proj_unit(self, wt, wk, sbank, sk, scol, sstart, M=128):
        cfg = self.cfg
        TP, NS = cfg.TP, cfg.NS
        u = self.next_unit()
        pp = self.PP[u % 3]
        pk = "PP%d" % (u % 3)
        for kc in range(NKC):
            for gi, (t0, n) in enumerate(cfg.big):
                self.mm(pp[0:M, gi * 512:gi * 512 + n], wt[:, kc, 0:M], self.H[:, kc, t0:t0 + n], kc == 0, kc == NKC - 1,
                        [wk, "H%d" % kc], [pk])
            self.mm(sbank[0:M, scol:scol + NS], wt[:, kc, 0:M], self.H[:, kc, TP:TP + NS],
                    kc == 0 and sstart, kc == NKC - 1, [wk, "H%d" % kc], [sk], skip_group_check=True)
        return pp, pk

    def down_block(self, w_rows, first, nrows=None):
        cfg = self.cfg
        TP, NS, NT = cfg.TP, cfg.NS, cfg.NT
        JB = len(w_rows)
        for jj in range(JB):
            r = w_rows[jj].shape[0]
            self.dma("pool", self.WD[0:r, jj, :], w_rows[jj], [], ["WD%d" % jj], "wd%d" % jj)
        for m in range(NKC):
            u = self.next_unit()
            pp = self.PP[u % 3]
            pk = "PP%d" % (u % 3)
            sbank = self.PS[m % 2]
            sk = "PS%d" % (m % 2)
            for jj in range(JB):
                r = w_rows[jj].shape[0]
                for gi, (t0, n) in enumerate(cfg.big):
                    self.mm(pp[:, gi * 512:gi * 512 + n], self.WD[0:r, jj, m * 128:(m + 1) * 128], self.ACTB[0:r, jj, t0:t0 + n],
                            jj == 0, jj == JB - 1, ["WD%d" % jj, "ACTB%d" % jj], [pk])
                self.mm(sbank[:, 0:NS], self.WD[0:r, jj, m * 128:(m + 1) * 128], self.ACTB[0:r, jj, TP:NT],
                        jj == 0, jj == JB - 1, ["WD%d" % jj, "ACTB%d" % jj], [sk])
            if first:
                self.cp("dve", self.Y[:, m, 0:TP], pp[:, 0:TP], [pk], ["Y%d" % m])
                self.cp("dve", self.Y[:, m, TP:NT], sbank[:, 0:NS], [sk], ["Y%d" % m])
            else:
                self.tt("dve", self.Y[:, m, 0:TP], pp[:, 0:TP], self.Y[:, m, 0:TP], ALU.add, [pk, "Y%d" % m], ["Y%d" % m])
                self.tt("dve", self.Y[:, m, TP:NT], sbank[:, 0:NS], self.Y[:, m, TP:NT], ALU.add, [sk, "Y%d" % m], ["Y%d" % m])

    def ffn(self, l, which):
        cfg = self.cfg
        NT, TP, NS = cfg.NT, cfg.TP, cfg.NS
        assert len(cfg.big) <= 2
        self.prenorm(l * 6 + (0 if which == 0 else 4))
        wgu = self.w_gu[l, which].rearrange("(kc p) n -> p kc n", p=128)
        wdn = self.w_dn[l, which]
        JB = 2
        NJ = DFF // 128
        for blk in range(NJ // JB):
            for jj in range(JB):
                j = blk * JB + jj
                sbank = self.PS[j % 2]
                sk = "PS%d" % (j % 2)
                res = []
                for gu in range(2):
                    c0 = gu * DFF + j * 128
                    wt, wk = self.load_wtile(wgu[:, :, c0:c0 + 128])
                    res.append(self.proj_unit(wt, wk, sbank, sk, gu * 8, gu == 0))
                (gp, gk), (up, uk) = res
                ts_ = j % 2
                self.act(self.TMP[:, ts_, 0:TP], gp[:, 0:TP], AF.Silu, [gk], ["TMP%d" % ts_])
                self.act(self.TMP[:, ts_, TP:NT], sbank[:, 0:NS], AF.Silu, [sk], ["TMP%d" % ts_])
                self.tt("dve", self.ACTB[:, jj, 0:TP], self.TMP[:, ts_, 0:TP], up[:, 0:TP], ALU.mult, ["TMP%d" % ts_, uk], ["ACTB%d" % jj])
                self.tt("dve", self.ACTB[:, jj, TP:NT], self.TMP[:, ts_, TP:NT], sbank[:, 8:8 + NS], ALU.mult, ["TMP%d" % ts_, sk], ["ACTB%d" % jj])
            self.down_block([wdn[(blk * JB + jj) * 128:(blk * JB + jj + 1) * 128, :] for jj in range(JB)], blk == 0)
        self.postnorm_add(self.GH, l * 6 + (1 if which == 0 else 5))

    def conv_mixer(self, l, ps_i):
        cfg = self.cfg
        NT, TP, NS = cfg.NT, cfg.TP, cfg.NS
        i = l // 2
        last = ps_i == cfg.NPASS - 1
        self.prenorm(l * 6 + 2)
        win = self.cv_w_in[i].rearrange("(kc p) n -> p kc n", p=128)
        wout = self.cv_w_out[i]
        self.load_fm_small(self.SCB, self.st_conv[i, ps_i * NS:(ps_i + 1) * NS].rearrange("s j d -> (s j) d"), NS * 2, D, ["SCB"])
        JB = 2
        cck = "CONVC%d" % i
        for blk in range(NKC // JB):
            for jj in range(JB):
                c = blk * JB + jj
                sbank = self.PS[c % 2]
                sk = "PS%d" % (c % 2)
                wt, wk = self.load_wtile(win[:, :, D + c * 128:D + (c + 1) * 128])
                cgp, cgk = self.proj_unit(wt, wk, sbank, sk, 0, True)
                wt, wk = self.load_wtile(win[:, :, 2 * D + c * 128:2 * D + (c + 1) * 128])
                hvp, hvk = self.proj_unit(wt, wk, sbank, sk, 8, False)
                wt, wk = self.load_wtile(win[:, :, c * 128:(c + 1) * 128])
                bgp, bgk = self.proj_unit(wt, wk, sbank, sk, 16, False)
                UB = self.TMP[:, 0, :]
                YB = self.TMP[:, 1, :]
                self.dma("sp", UB[:, 0:2], self.CONVC[i, :, c, :], [cck], ["TMP0"], "ccin")
                self.cp("act", UB[:, 2:2 + TP], cgp[:, 0:TP], [cgk], ["TMP0"])
                self.cp("act", UB[:, 2 + TP:2 + NT], sbank[:, 0:NS], [sk], ["TMP0"])
                self.tt("dve", UB[:, 2:2 + TP], UB[:, 2:2 + TP], hvp[:, 0:TP], ALU.mult, ["TMP0", hvk], ["TMP0"])
                self.tt("dve", UB[:, 2 + TP:2 + NT], UB[:, 2 + TP:2 + NT], sbank[:, 8:8 + NS], ALU.mult, ["TMP0", sk], ["TMP0"])
                self.dma("sp", self.CONVC[i, :, c, :], UB[:, TP:TP + 2], ["TMP0"], [cck], "ccout")
                w0 = self.CW[:, i * 3 + 0, c:c + 1]
                w1 = self.CW[:, i * 3 + 1, c:c + 1]
                w2 = self.CW[:, i * 3 + 2, c:c + 1]
                self.ts("dve", YB[:, 0:TP], UB[:, 2:2 + TP], w2, None, ALU.mult, None, ["TMP0", "CW"], ["TMP1"])
                self.stt("dve", YB[:, 0:TP], UB[:, 1:1 + TP], w1, YB[:, 0:TP], ALU.mult, ALU.add, ["TMP0", "CW", "TMP1"], ["TMP1"])
                self.stt("dve", YB[:, 0:TP], UB[:, 0:TP], w0, YB[:, 0:TP], ALU.mult, ALU.add, ["TMP0", "CW", "TMP1"], ["TMP1"])
                scb = self.SCB[:, :, c].rearrange("p (s j) -> p s j", j=2)
                sout = self.SOUT[:, :, c].rearrange("p (s j) -> p s j", j=2)
                self.ts("dve", YB[:, TP:NT], UB[:, 2 + TP:2 + NT], w2, None, ALU.mult, None, ["TMP0", "CW"], ["TMP1"])
                self.cp("dve", self.STAT[:, 0:NS], scb[:, :, 0], ["SCB"], ["STAT"])
                self.cp("dve", self.STAT[:, NS:2 * NS], scb[:, :, 1], ["SCB"], ["STAT"])
                self.stt("dve", YB[:, TP:NT], self.STAT[:, NS:2 * NS], w1, YB[:, TP:NT], ALU.mult, ALU.add, ["STAT", "CW", "TMP1"], ["TMP1"])
                self.stt("dve", YB[:, TP:NT], self.STAT[:, 0:NS], w0, YB[:, TP:NT], ALU.mult, ALU.add, ["STAT", "CW", "TMP1"], ["TMP1"])
                self.cp("dve", sout[:, :, 0], scb[:, :, 1], ["SCB"], ["SOUT"])
                self.cp("dve", sout[:, :, 1], UB[:, 2 + TP:2 + NT], ["TMP0"], ["SOUT"])
                self.tt("dve", self.ACTB[:, jj, 0:TP], YB[:, 0:TP], bgp[:, 0:TP], ALU.mult, ["TMP1", bgk], ["ACTB%d" % jj])
                self.tt("dve", self.ACTB[:, jj, TP:NT], YB[:, TP:NT], sbank[:, 16:16 + NS], ALU.mult, ["TMP1", sk], ["ACTB%d" % jj])
            self.down_block([wout[(blk * JB + jj) * 128:(blk * JB + jj + 1) * 128, :] for jj in range(JB)], blk == 0)
        self.postnorm_add(self.G, l * 6 + 3)
        self.fence(self.ky(), ["YR"])
        self.store_fm_small(self.s_conv[i, ps_i * NS:(ps_i + 1) * NS].rearrange("s j d -> (s j) d"), self.SOUT, NS * 2, ["SOUT"])
        if last:
            cc = self.sb_at("CCS%d" % self.next_unit(), [128, NKC, 2], F32, self.TMP_off)
            self.dma("sp", cc[:], self.CONVC[i], [cck], ["TMP0"], "ccin")
            stg = self.sb_at("PSTO%d" % self.next_unit(), [2, D], F32, self.Y_off + 8192)
            for c in range(NKC):
                ps = self.PS[c % 2]
                self.tr(ps[0:2, 0:128], cc[:, c, :], self.IDENT[:, :], ["TMP0", "IDENT"], ["PS%d" % (c % 2)])
                self.cp("dve", stg[:, c * 128:(c + 1) * 128], ps[0:2, 0:128], ["PS%d" % (c % 2)], ["YR"])
            self.dma("sp", self.p_conv[i], stg[:], ["YR"], [], "pconv%d" % i)
        self.fence(["YR"], self.ky() + ["STG0", "STG1"])

    def load_fm_small(self, dst, src_rows, R, ncols, keys_w):
        self.fence(self.ky(), ["YR"])
        stg = self.sb_at("PSTG%d" % self.next_unit(), [R, ncols], F32, self.Y_off)
        self.dma("sp", stg[:], src_rows, [], ["YR"], "fmsmall")
        nch = (ncols + 127) // 128
        for c in range(nch):
            n = min(128, ncols - c * 128)
            ps = self.PS[c % 2]
            self.tr(ps[0:n, 0:R], stg[:, c * 128:c * 128 + n], self.IDENT[0:R, 0:R], ["YR", "IDENT"], ["PS%d" % (c % 2)])
            self.cp("dve", dst[0:n, :, c], ps[0:n, 0:R], ["PS%d" % (c % 2)], keys_w)
        self.fence(["YR"], self.ky())

    def store_fm_small(self, dst_rows, src, R, keys_r):
        ncols = dst_rows.shape[1]
        stg = self.sb_at("PSTS%d" % self.next_unit(), [R, ncols], F32, self.Y_off)
        nch = (ncols + 127) // 128
        for c in range(nch):
            n = min(128, ncols - c * 128)
            ps = self.PS[c % 2]
            self.tr(ps[0:R, 0:n], src[0:n, :, c], self.IDENT[0:n, 0:n], keys_r + ["IDENT"], ["PS%d" % (c % 2)])
            self.cp("dve", stg[:, c * 128:c * 128 + n], ps[0:R, 0:n], ["PS%d" % (c % 2)], ["YR"])
        self.dma("sp", dst_rows, stg[:], ["YR"], [], "fmsmall")

    def load_wtile2(self, src_a, src_b, n):
        slot = self.wslot % 4
        self.wslot += 1
        wt = self.WG[:, slot, :].rearrange("p (kc n) -> p kc n", n=128)
        self.dma("pool", wt[:, :, 0:n], src_a, [], ["WG%d" % slot], "wg%d" % slot)
        self.dma("pool", wt[:, :, n:2 * n], src_b, [], ["WG%d" % slot], "wg%d" % slot)
        return wt, "WG%d" % slot

    def rope(self, pp, pk, sbank, sk, dst_big, dst_small, dkeys, keep_f32):
        cfg = self.cfg
        TP, NS, NT = cfg.TP, cfg.NS, cfg.NT
        R0 = self.TMP[:, 0, 0:NT]
        R1 = self.TMP[:, 1, 0:NT]
        self.cp("act", R0[:, 0:TP], pp[:, 0:TP], [pk], ["TMP0"])
        self.cp("act", R0[:, TP:NT], sbank[:, 0:NS], [sk], ["TMP0"])
        for (a, b) in ((0, 32), (32, 0), (64, 96), (96, 64)):
            self.cp("dve", R1[a:a + 32, :], R0[b:b + 32, :], ["TMP0"], ["TMP1"])
        self.tt("dve", R0, R0, self.COS[:, :], ALU.mult, ["TMP0", "ROPE"], ["TMP0"])
        self.tt("dve", R1, R1, self.SIN[:, :], ALU.mult, ["TMP1", "ROPE"], ["TMP1"])
        self.tt("dve", R0, R0, R1, ALU.add, ["TMP0", "TMP1"], ["TMP0"])
        self.cp("act", dst_big, R0[:, 0:TP], ["TMP0"], dkeys)
        self.cp("act", dst_small, R0[:, TP:NT], ["TMP0"], dkeys)

    def ab_mixer(self, l, ps_i):
        cfg = self.cfg
        TP, NS, NT = cfg.TP, cfg.NS, cfg.NT
        NB = TP // 128
        i = l // 2
        last = ps_i == cfg.NPASS - 1
        s0 = ps_i * NS
        self.prenorm(l * 6 + 2)
        win = self.ab_w_in[i].rearrange("(kc p) n -> p kc n", p=128)
        wout = self.ab_w_out[i].rearrange("(kc p) n -> p kc n", p=128)
        self.ovk = (["QT%d" % c for c in range(8)] + ["KT%d" % c for c in range(4)] + ["KTc", "VCs", "KCs", "ROPE", "MASK", "SSB",
                    "F4a", "F4b", "YR", "RW", "VSA"] + ["VT%d" % c for c in range(NB + 1)])
        self.fence(self.ky() + ["STG0", "STG1"], self.ovk)
        yo = self.Y_off
        ylim = self.Y_off + NKC * NT * 4

        def ytake(name, shape, dt, nbytes):
            nonlocal yo
            yo = (yo + 31) // 32 * 32
            t = self.sb_at("%s_%d" % (name, self.next_unit()), shape, dt, yo)
            yo += nbytes
            assert yo <= ylim, (name, yo, ylim)
            return t
        NTP = NT + 32
        QT = ytake("QT", [128, 8, NTP], BF16, 8 * NTP * 2)
        self.memset("dve", QT[:, :, NT:NTP], 0.0, ["QT%d" % c for c in range(8)])
        KT = ytake("KT", [128, 4, 128 + NT], BF16, 4 * (128 + NT) * 2)
        VT = ytake("VT", [128, NB + 1, 256], BF16, (NB + 1) * 512)
        VCs = ytake("VCs", [128, 256], BF16, 512)
        KCs = ytake("KCs", [128, 4, 128], BF16, 1024)
        self.COS = ytake("COS", [128, NT], F32, NT * 4)
        self.SIN = ytake("SIN", [128, NT], F32, NT * 4)
        MASK = ytake("MASK", [128, 2, 256], F32, 2048)
        SSB = ytake("SSB", [128, 4, 256], F32, 4096)
        F4 = ytake("F4", [128, 2, 256], F32, 2048)
        VSA = ytake("VSA", [128, 256], F32, 1024)
        self.dma("sp", self.COS[:], self.rope_t[ps_i, 0], [], ["ROPE"], "ropeld")
        self.dma("sp", self.SIN[:], self.rope_t[ps_i, 1], [], ["ROPE"], "ropeld2")
        self.dma("sp", MASK[:], self.masks.rearrange("m q k -> q m k"), [], ["MASK"], "maskld")
        self.dma("sp", VT[:, 0, :], self.VCAR[i], ["VCAR%d" % i], ["VT0"], "vcin")
        self.dma("sp", KT[:, :, 0:128], self.KCAR[i], ["KCAR%d" % i], ["KTc"], "kcin")

        import os
        stop = int(os.environ.get("AB_STOP", "9"))
        wv = []
        for hf in range(2):
            wv.append(self.load_wtile(win[:, :, 1280 + hf * 128:1280 + (hf + 1) * 128]))
        on = lambda k: stop >= k
        if on(3):
            self.memset("dve", VSA[:, :], 0.0, ["VSA"])
        for hf in range(2 if on(2) else 0):
            wt, wk = wv[hf]
            sb_ = self.PS[hf]
            sk = "PS%d" % hf
            pp, pk = self.proj_unit(wt, wk, sb_, sk, 0, True)
            R = self.TMP[:, hf, 0:NT]
            tk = "TMP%d" % hf
            self.cp("act", R[:, 0:TP], pp[:, 0:TP], [pk], [tk])
            self.cp("act", R[:, TP:NT], sb_[:, 0:NS], [sk], [tk])
            pt = self.PS[1 - hf]
            ptk = "PS%d" % (1 - hf)
            for tb in range(NB if not os.environ.get("NO_VTR") else 0):
                self.tr(pt[:, 0:128], R[:, tb * 128:(tb + 1) * 128], self.IDENT[:, :], [tk, "IDENT"], [ptk])
                if not os.environ.get("NO_VTCP"):
                    self.cp("dve", VT[:, tb + 1, hf * 128:(hf + 1) * 128], pt[:, 0:128], [ptk], ["VT%d" % (tb + 1)])
                if last and tb == NB - 1 and not os.environ.get("NO_VTCP"):
                    self.cp("dve", F4[:, 0, hf * 128:(hf + 1) * 128], pt[:, 0:128], [ptk], ["F4a"])
            if on(3):
                self.tr(pt[0:NS, 128:256], R[:, TP:NT], self.IDENT[:, :], [tk, "IDENT"], [ptk])
                self.cp("dve", VSA[0:NS, hf * 128:(hf + 1) * 128], pt[0:NS, 128:256], [ptk], ["VSA"])
        if last and on(2) and not os.environ.get("NO_VDMA"):
            self.dma("sp", self.p_swa_v[i], F4[:, 0, :], ["F4a"], [], "psv%d" % i)
        for s in range(NS if (on(3) and not os.environ.get("NO_VDMA")) else 0):
            self.dma("sp", self.s_swa_v[i, s0 + s, 127:128, :], VSA[s:s + 1, :], ["VSA"], [], "vsaout")
            self.dma("sp", self.s_swa_v[i, s0 + s, 0:127, :], self.cache_v[i, s0 + s, 1:128, :], [], [], "cachecp")
            self.dma("sp", self.s_swa_k[i, s0 + s, 0:127, :], self.cache_k[i, s0 + s, 1:128, :], [], [], "cachecp")
        self.dma("sp", self.VCAR[i], VT[:, NB, :], ["VT%d" % NB], ["VCAR%d" % i], "vcout")

        KSO = F4[:, 1, :]
        for kh in range(4 if on(4) else 0):
            sb_ = self.PS[kh % 2]
            sk = "PS%d" % (kh % 2)
            ksrc = win[:, :, 1024 + kh * 64:1024 + (kh + 1) * 64]
            wt, wk = self.load_wtile2(ksrc, ksrc, 64)
            pp, pk = self.proj_unit(wt, wk, sb_, sk, 0, True)
            self.rope(pp, pk, sb_, sk, KT[:, kh, 128:128 + TP], KT[:, kh, 128 + TP:128 + NT], ["KT%d" % kh], True)
            R0 = self.TMP[:, 0, 0:NT]
            if last:
                pt = self.PS[(kh + 1) % 2]
                ptk = "PS%d" % ((kh + 1) % 2)
                self.tr(pt[:, 0:64], R0[0:64, TP - 128:TP], self.IDENT[0:64, 0:64], ["TMP0", "IDENT"], [ptk])
                self.cp("dve", F4[:, 0, kh * 64:(kh + 1) * 64], pt[:, 0:64], [ptk], ["F4a"])
            pt = self.PS[(kh + 1) % 2]
            ptk = "PS%d" % ((kh + 1) % 2)
            self.tr(pt[0:NS, 64:128], R0[0:64, TP:NT], self.IDENT[0:64, 0:64], ["TMP0", "IDENT"], [ptk])
            self.cp("dve", KSO[0:NS, kh * 64:(kh + 1) * 64], pt[0:NS, 64:128], [ptk], ["F4b"])
            for cq in (2 * kh, 2 * kh + 1):
                wt, wk = self.load_wtile(win[:, :, cq * 128:(cq + 1) * 128])
                pp, pk = self.proj_unit(wt, wk, sb_, sk, 8, True)
                self.rope_q(pp, pk, sb_, sk, QT, cq)
        if last:
            self.dma("sp", self.p_swa_k[i], F4[:, 0, :], ["F4a"], [], "psk%d" % i)
        for s in range(NS):
            self.dma("sp", self.s_swa_k[i, s0 + s, 127:128, :], KSO[s:s + 1, :], ["F4b"], [], "f4bout2")
        for kh in range(4):
            self.dma("sp", self.KCAR[i][:, kh, :], KT[:, kh, TP:TP + 128], ["KT%d" % kh], ["KCAR%d" % i], "kcout%d" % kh)

        if cfg.mixers >= 3 and on(5):
            self.rwkv_proj(l, ps_i, win)

        import os
        if on(6):
            self.swa_prompt(i, ps_i, QT, KT, VT, MASK, SSB)
        if on(7):
            self.swa_sample(i, ps_i, QT, KT, VCs, KCs, SSB, F4, VSA)
        if cfg.mixers >= 3 and on(8):
            self.rwkv_main(l, ps_i)
        else:
            for c in range(8, 16):
                self.memset("dve", self.H[:, c, :], 0.0, ["H%d" % c])
        self.fence(self.ovk, self.ky() + ["STG0", "STG1"])
        for m in range(NKC):
            sb_ = self.PS[m % 2]
            sk = "PS%d" % (m % 2)
            wt, wk = self.load_wtile(wout[:, :, m * 128:(m + 1) * 128])
            pp, pk = self.proj_unit(wt, wk, sb_, sk, 0, True)
            self.cp("act", self.Y[:, m, 0:TP], pp[:, 0:TP], [pk], ["Y%d" % m])
            self.cp("act", self.Y[:, m, TP:NT], sb_[:, 0:NS], [sk], ["Y%d" % m])
        self.postnorm_add(self.G, l * 6 + 3)

    def store_fm_chunks(self, dst_rows, src, R, ncols, skeys):
        nch = (ncols + 127) // 128
        for g0 in range(0, nch, 4):
            ps = self.PS[(g0 // 4) % 2]
            pk = "PS%d" % ((g0 // 4) % 2)
            w = 0
            for c in range(g0, min(nch, g0 + 4)):
                n = min(128, ncols - c * 128)
                self.tr(ps[0:R, (c - g0) * 128:(c - g0) * 128 + n], src[0:n, :, c], self.IDENT[0:n, 0:n], skeys + ["IDENT"], [pk])
                w += n
            st = self.TMP[0:R, 1, 0:w]
            self.cp("dve", st, ps[0:R, 0:w], [pk], ["TMP1"])
            self.dma("sp", dst_rows[:, g0 * 128:g0 * 128 + w], st, ["TMP1"], [], "fmchunk")

    def rwkv_proj(self, l, ps_i, win):
        cfg = self.cfg
        TP, NS, NT = cfg.TP, cfg.NS, cfg.NT
        i = l // 2
        last = ps_i == cfg.NPASS - 1
        s0 = ps_i * NS
        shk = "SHCAR%d" % i
        for cc in range(27):
            M = 128 if cc < 26 else 32
            c0 = A_COLS + cc * 128
            sb_ = self.PS[cc % 2]
            sk = "PS%d" % (cc % 2)
            wt, wk = self.load_wtile(win[:, :, c0:c0 + M], ncol=M)
            pp, pk = self.proj_unit(wt, wk, sb_, sk, 0, True, M=128)
            tsl = cc % 2
            stg = self.TMP[:, tsl, 0:NT]
            tk = "TMP%d" % tsl
            self.cp("act", stg[0:M, 0:TP], pp[0:M, 0:TP], [pk], [tk])
            self.cp("act", stg[0:M, TP:NT], sb_[0:M, 0:NS], [sk], [tk])
            self.dma("sp", self.PBD[cc, 0:M, 1:1 + NT], stg[0:M, :], [tk], ["PBD%d" % cc], "pbd%d" % tsl)
            self.dma("sp", self.PBD[cc, 0:M, 0:1], self.SHCAR[i][0:M, cc:cc + 1], [shk], ["PBD%d" % cc], "pbdc", allow_slow_non_contiguous=True)
            self.dma("sp", self.SHCAR[i][0:M, cc:cc + 1], stg[0:M, TP - 1:TP], [tk], [shk], "shc%d" % tsl, allow_slow_non_contiguous=True)
            self.cp("dve", self.SHS[0:M, :, cc], stg[0:M, TP:NT], [tk], ["SHS"])
        self.store_fm_chunks(self.s_shift[i, s0:s0 + NS, :], self.SHS, NS, B_COLS, ["SHS"])
        if last:
            self.dma("sp", self.SHP[:, 0, :], self.SHCAR[i], [shk], ["SHP"], "shpld")
            self.store_fm_chunks(self.p_shift[i:i + 1, :], self.SHP, 1, B_COLS, ["SHP"])

    def fp32_cols_mm(self, lhsT, rhs_tile, rows, rkeys, accumulate=None):
        cfg = self.cfg
        TP, NS = cfg.TP, cfg.NS
        u = self.next_unit()
        pp = self.PP[u % 3]
        pk = "PP%d" % (u % 3)
        sb_ = self.PS[u % 2]
        sk = "PS%d" % (u % 2)
        ops = [(lhsT, rhs_tile, rows)] + (accumulate or [])
        for oi, (lt, rt, rw) in enumerate(ops):
            for gi, (t0, n) in enumerate(cfg.big):
                self.mm(pp[:, gi * 512:gi * 512 + n], lt, rt[rw, t0:t0 + n], oi == 0, oi == len(ops) - 1, rkeys, [pk])
            self.mm(sb_[:, 0:NS], lt, rt[rw, TP:TP + NS], oi == 0, oi == len(ops) - 1, rkeys, [sk])
        return pp, pk, sb_, sk

    def rwkv_main(self, l, ps_i):
        cfg = self.cfg
        TP, NS, NT = cfg.TP, cfg.NS, cfg.NT
        i = l // 2
        last = ps_i == cfg.NPASS - 1
        s0 = ps_i * NS
        P7 = i * 7
        par = lambda k, hp: self.PAR[:, P7 + k, hp:hp + 1]
        rwk = ["PBX", "CM24", "CM25", "CM26", "WSM", "B1", "B2", "B3", "B4", "B5", "SHST"]
        self.fence(self.ovk, rwk)
        yo = self.Y_off
        ylim = self.Y_off + NKC * NT * 4

        def ytake(name, shape, dt, nbytes):
            nonlocal yo
            yo = (yo + 31) // 32 * 32
            t = self.sb_at("%s_%d" % (name, self.next_unit()), shape, dt, yo)
            yo += nbytes
            assert yo <= ylim, (name, yo, ylim)
            return t
        PBX = ytake("PBX", [128, 1 + NT], F32, (1 + NT) * 4)
        CM24 = ytake("CM24", [128, NT], F32, NT * 4)
        CM25 = ytake("CM25", [128, NT], F32, NT * 4)
        CM26 = ytake("CM26", [128, NT], F32, NT * 4)
        WSM1 = ytake("WSM1", [128, 1024], F32, 4096)
        WSM2 = ytake("WSM2", [128, 1024], F32, 4096)
        WSM3 = ytake("WSM3", [128, 1024], F32, 4096)
        Bs = [ytake("B%d" % k, [128, NT], F32, NT * 4) for k in range(1, 6)]
        SHST = ytake("SHST", [128, NS, 27], F32, NS * 27 * 4)
        self.dma("sp", WSM1[0:64, :], self.rw_wdec[i], [], ["WSM"], "wsm1")
        self.dma("sp", WSM1[64:128, :], self.rw_waaa[i], [], ["WSM"], "wsm2")
        self.dma("sp", WSM2[:, :], self.rw_wgate[i, 0:128, :], [], ["WSM"], "wsm3")
        self.memset("dve", WSM3[:, :], 0.0, ["WSM"])
        self.memset("dve", CM26[:, :], 0.0, ["CM26"])
        self.dma("sp", WSM3[0:32, :], self.rw_wgate[i, 128:160, :], [], ["WSM"], "wsm4")
        stk = ["WD0", "WD1", "ACTB0", "ACTB1", "TMP0", "TMP1"]
        stg = self.sb_at("SHSTG_%d" % self.next_unit(), [NS, B_COLS], F32, self.WD_off)
        self.dma("sp", stg[:], self.st_shift[i, s0:s0 + NS, :], [], stk, "shstg")
        self.memset("dve", SHST[:], 0.0, ["SHST"])
        for c in range(27):
            n = min(128, B_COLS - c * 128)
            ps = self.PS[c % 2]
            self.tr(ps[0:n, 0:NS], stg[:, c * 128:c * 128 + n], self.IDENT[0:NS, 0:NS], stk + ["IDENT"], ["PS%d" % (c % 2)])
            self.cp("dve", SHST[0:n, :, c], ps[0:n, 0:NS], ["PS%d" % (c % 2)], ["SHST"])

        def lerp(dst, dk, cc, M=128):
            mu = self.MU[0:M, i, cc:cc + 1]
            omu = self.OMU[0:M, i, cc:cc + 1]
            self.dma("sp", PBX[0:M, :], self.PBD[cc, 0:M, :], ["PBD%d" % cc], ["PBX"], "pbx")
            self.ts("dve", dst[0:M, 0:TP], PBX[0:M, 1:1 + TP], omu, None, ALU.mult, None, ["PBX", "OMU"], [dk])
            self.stt("dve", dst[0:M, 0:TP], PBX[0:M, 0:TP], mu, dst[0:M, 0:TP], ALU.mult, ALU.add, ["PBX", "MU", dk], [dk])
            self.ts("dve", dst[0:M, TP:NT], PBX[0:M, 1 + TP:1 + NT], omu, None, ALU.mult, None, ["PBX", "OMU"], [dk])
            self.cp("dve", self.STAT[0:M, 0:NS], SHST[0:M, :, cc], ["SHST"], ["STAT"])
            self.stt("dve", dst[0:M, TP:NT], self.STAT[0:M, 0:NS], mu, dst[0:M, TP:NT], ALU.mult, ALU.add, ["STAT", "MU", dk], [dk])

        lerp(CM24, "CM24", 24)
        self.act(CM24[0:64, :], CM24[0:64, :], AF.Tanh, ["CM24"], ["CM24"])
        lerp(CM25, "CM25", 25)
        self.act(CM25[:, :], CM25[:, :], AF.Sigmoid, ["CM25"], ["CM25"])
        lerp(CM26, "CM26", 26, M=32)
        self.act(CM26[0:32, :], CM26[0:32, :], AF.Sigmoid, ["CM26"], ["CM26"])
        B1, B2, B3, B4, B5 = Bs

        def evac(dst, dk, pp, pk, sb_, sk, func=None, bias=None, scale=None):
            if func is None:
                self.cp("act", dst[:, 0:TP], pp[:, 0:TP], [pk], [dk])
                self.cp("act", dst[:, TP:NT], sb_[:, 0:NS], [sk], [dk])
            else:
                self.act(dst[:, 0:TP], pp[:, 0:TP], func, [pk, "PAR"], [dk], bias=bias, scale=scale)
                self.act(dst[:, TP:NT], sb_[:, 0:NS], func, [sk, "PAR"], [dk], bias=bias, scale=scale)

        def out(q, hp, src, sk_):
            self.dma("sp", self.RWQ[q, :, hp, :], src[:, :], [sk_], ["RWQ%d" % q], "rwq_%s" % sk_)

        for hp in range(8):
            hs = slice(hp * 128, (hp + 1) * 128)
            lerp(B1, "B1", 8 + hp)
            r_ = self.fp32_cols_mm(WSM1[64:128, hs], CM24, slice(64, 128), ["WSM", "CM24"])
            evac(B2, "B2", *r_, func=AF.Sigmoid, bias=par(1, hp))
            self.ts("dve", B3[:, :], B1[:, :], par(2, hp), None, ALU.mult, None, ["B1", "PAR"], ["B3"])
            self.tt("dve", B4[:, :], B3[:, :], B3[:, :], ALU.mult, ["B3"], ["B4"])
            r_ = self.fp32_cols_mm(self.BONES[:, :], B4, slice(0, 128), ["BONES", "B4"])
            evac(B4, "B4", *r_, func=AF.Sqrt)
            self.ts("dve", B4[:, :], B4[:, :], 1e-12, None, ALU.max, None, ["B4"], ["B4"])
            self.recip(B4[:, :], B4[:, :], ["B4"], ["B4"])
            self.tt("dve", B3[:, :], B3[:, :], B4[:, :], ALU.mult, ["B3", "B4"], ["B3"])
            out(1, hp, B3, "B3")
            self.stt("dve", B4[:, :], B3[:, :], -1.0, B2[:, :], ALU.mult, ALU.mult, ["B3", "B2"], ["B4"])
            out(2, hp, B4, "B4")
            self.ts("dve", B5[:, :], B2[:, :], -1.0, par(3, hp), ALU.add, ALU.mult, ["B2", "PAR"], ["B5"])
            self.stt("dve", B5[:, :], B5[:, :], 1.0, B1[:, :], ALU.add, ALU.mult, ["B5", "B1"], ["B5"])
            out(3, hp, B5, "B5")
            lerp(B1, "B1", hp)
            out(0, hp, B1, "B1")
            lerp(B2, "B2", 16 + hp)
            out(5, hp, B2, "B2")
            r_ = self.fp32_cols_mm(WSM1[0:64, hs], CM24, slice(0, 64), ["WSM", "CM24"])
            evac(B3, "B3", *r_, func=AF.Sigmoid, bias=par(0, hp))
            self.act(B3[:, :], B3[:, :], AF.Exp, ["B3"], ["B3"], scale=-0.6065306597126334)
            out(4, hp, B3, "B3")
            r_ = self.fp32_cols_mm(WSM2[:, hs], CM25, slice(0, 128), ["WSM", "CM25", "CM26"],
                                   accumulate=[(WSM3[:, hs], CM26, slice(0, 128))])
            evac(B4, "B4", *r_)
            out(6, hp, B4, "B4")

        TBK = 64 if NT >= 1024 else 32
        scank = ["QB", "QBT", "VTM", "Z", "ZW", "T1", "T2", "YST", "SL", "ZS", "QS", "QST", "VTS", "SO"]
        self.fence(rwk, scank)
        yo = self.Y_off
        QB = [ytake("QB%d" % q, [128, 8, TBK], F32, 8 * TBK * 4) for q in range(6)]
        QBT = {q: ytake("QBT%d" % q, [128, TBK, 8], F32, 8 * TBK * 4) for q in (2, 3, 4)}
        VTM = ytake("VTM", [64, 1024], F32, 4096)
        Z = ytake("Z", [128, 8, 64], F32, 2048)
        ZW = ytake("ZW", [128, 8, 64], F32, 2048)
        T1 = ytake("T1", [128, 8, 64], F32, 2048)
        T2 = ytake("T2", [128, 8, 64], F32, 2048)
        YST = ytake("YST", [128, 8, TBK], F32, 8 * TBK * 4)
        PVB = [self.PP[0][:, 0:512], self.PP[0][:, 512:1024]]
        PUB = [self.PP[1][:, 0:512], self.PP[1][:, 512:1024]]
        PYh = [self.PP[2][:, h2 * 512:h2 * 512 + 8 * TBK].rearrange("p (h t) -> p h t", t=TBK) for h2 in range(2)]

        def step(Zt, zk, kk_t, r_t, wT, nbT, kpT, vtm, nrows, trow, pyo, par_, qkeys):
            pvb = PVB[par_]
            vk = "PVB%d" % par_
            v4 = vtm.rearrange("p (hp h2 v) -> p hp h2 v", hp=8, h2=2)
            for h2 in range(2):
                self.mm(pvb[h2 * 64:(h2 + 1) * 64, :], self.IDENT[0:nrows, trow:trow + 1].to_broadcast([nrows, 64]), v4[:, :, h2, :],
                        True, True, ["IDENT"] + qkeys, [vk])
            for h2 in range(2):
                hs_ = slice(h2 * 64, (h2 + 1) * 64)
                for hp in range(8):
                    self.mm(PUB[h2][hs_, hp * 64:(hp + 1) * 64], kk_t[hs_, hp, :].to_broadcast([64, 64]), Zt[hs_, hp, :], True, True,
                            [zk] + qkeys, ["PUB%d" % h2], skip_group_check=True)
            bc = lambda a: a.unsqueeze(2).to_broadcast([a.shape[0], 8, 64])
            p3 = lambda a: a.rearrange("p (h v) -> p h v", h=8)
            self.tt("dve", ZW[:, :, :], Zt[:, :, :], bc(wT), ALU.mult, [zk] + qkeys, ["ZW"])
            for h2 in range(2):
                hs_ = slice(h2 * 64, (h2 + 1) * 64)
                self.tt("dve", T1[hs_, :, :], p3(PUB[h2][hs_, :]), bc(nbT[hs_, :]), ALU.mult, ["PUB%d" % h2] + qkeys, ["T1"])
            self.tt("dve", T2[:, :, :], p3(pvb), bc(kpT), ALU.mult, [vk] + qkeys, ["T2"])
            self.tt("dve", ZW[:, :, :], ZW[:, :, :], T1[:, :, :], ALU.add, ["ZW", "T1"], ["ZW"])
            self.tt("dve", Zt[:, :, :], ZW[:, :, :], T2[:, :, :], ALU.add, ["ZW", "T2"], [zk])
            for h2 in range(2):
                hs_ = slice(h2 * 64, (h2 + 1) * 64)
                for hp in range(8):
                    self.mm(pyo(h2, hs_, hp), Zt[hs_, hp, :], r_t[hs_, hp, :], True, True, [zk] + qkeys, ["PY%d" % h2], skip_group_check=True)

        self.dma("sp", Z[:, :, :], self.ZCAR[i], ["ZCAR%d" % i], ["Z"], "zin")
        for b in range(TP // TBK):
            c0 = b * TBK
            for q in range(6):
                self.dma("sp", QB[q][:, :, :], self.RWQ[q, :, :, c0:c0 + TBK], ["RWQ%d" % q], ["QB"], "qb%d" % q)
            for q in (2, 3, 4):
                self.cp("pool", QBT[q][:, :, :], QB[q][:, :, :].rearrange("p h t -> p t h"), ["QB"], ["QBT"])
            for g0 in range(0, 8, 4):
                ps = self.PS[(g0 // 4) % 2]
                pk = "PS%d" % ((g0 // 4) % 2)
                for hp in range(g0, g0 + 4):
                    self.tr(ps[0:TBK, (hp - g0) * 128:(hp - g0 + 1) * 128], QB[5][:, hp, :], self.IDENT[:, :], ["QB", "IDENT"], [pk])
                self.cp("act", VTM[0:TBK, g0 * 128:(g0 + 4) * 128], ps[0:TBK, 0:512], [pk], ["VTM"])
            for t in range(TBK):
                step(Z, "Z", QB[1][:, :, t:t + 1], QB[0][:, :, t:t + 1], QBT[4][:, t, :], QBT[2][:, t, :], QBT[3][:, t, :],
                     VTM[0:TBK, :], TBK, t, (lambda h2, hs_, hp, t=t: PYh[h2][hs_, hp, t:t + 1]), t % 2, ["QB", "QBT", "VTM"])
            self.cp("act", YST[0:64, :, :], PYh[0][0:64, :, :], ["PY0"], ["YST"])
            self.cp("act", YST[64:128, :, :], PYh[1][64:128, :, :], ["PY1"], ["YST"])
            self.dma("sp", self.YSD[:, :, c0:c0 + TBK], YST[:, :, :], ["YST"], ["YSD"], "ysd")
        self.dma("sp", self.ZCAR[i], Z[:, :, :], ["Z"], ["ZCAR%d" % i], "zout")
        if last:
            SO = VTM[:, :].rearrange("p (h k) -> p h k", h=8)
            for g0 in range(0, 8, 4):
                ps = self.PS[(g0 // 4) % 2]
                pk = "PS%d" % ((g0 // 4) % 2)
                for hp in range(g0, g0 + 4):
                    self.tr(ps[0:64, (hp - g0) * 128:(hp - g0 + 1) * 128], Z[:, hp, :], self.IDENT[:, :], ["Z", "IDENT"], [pk])
                self.cp("act", SO[:, g0:g0 + 4, :], ps[0:64, 0:512].rearrange("p (h k) -> p h k", h=4), [pk], ["VTM"])
            self.dma("sp", self.p_rwkv[i].rearrange("(hp h2) v k -> v hp h2 k", h2=2), SO[:, :, :].rearrange("p h (a k) -> p h a k", a=2),
                     ["VTM"], [], "prw%d" % i)
        QS = [ytake("QS%d" % q, [128, 8, NS], F32, 8 * NS * 4) for q in range(6)]
        QST = {q: ytake("QST%d" % q, [128, NS, 8], F32, 8 * NS * 4) for q in (2, 3, 4)}
        VTS = VTM
        SL = ytake("SL", [64, 16, 64], F32, 4096)
        ZS = ytake("ZS", [128, 8, 64], F32, 2048)
        SOs = SL[:, :, :].rearrange("p (h a) k -> p h (a k)", a=2)
        for q in range(6):
            self.dma("sp", QS[q][:, :, :], self.RWQ[q, :, :, TP:TP + NS], ["RWQ%d" % q], ["QS"], "qs%d" % q)
        for q in (2, 3, 4):
            self.cp("pool", QST[q][:, :, :], QS[q][:, :, :].rearrange("p h t -> p t h"), ["QS"], ["QST"])
        self.memset("dve", VTM[:, :], 0.0, ["VTM"])
        for g0 in range(0, 8, 4):
            ps = self.PS[(g0 // 4) % 2]
            pk = "PS%d" % ((g0 // 4) % 2)
            for hp in range(g0, g0 + 4):
                self.tr(ps[0:NS, (hp - g0) * 128:(hp - g0 + 1) * 128], QS[5][:, hp, :], self.IDENT[:, :], ["QS", "IDENT"], [pk])
            self.cp("act", VTS[0:NS, g0 * 128:(g0 + 4) * 128], ps[0:NS, 0:512], [pk], ["VTM"])
        PYSh = [self.PP[2][:, h2 * 512:h2 * 512 + 8 * NS].rearrange("p (h t) -> p h t", t=NS) for h2 in range(2)]
        for s in range(NS):
            self.dma("sp", SL[:, :, :], self.st_rwkv[i, s0 + s].rearrange("h v k -> v h k"), [], ["SL"], "slld")
            for h2 in range(2):
                for g0 in range(0, 8, 4):
                    u = self.next_unit()
                    ps = self.PS[u % 2]
                    pk = "PS%d" % (u % 2)
                    for hp in range(g0, g0 + 4):
                        self.tr(ps[0:64, (hp - g0) * 64:(hp - g0 + 1) * 64], SL[:, 2 * hp + h2, :], self.IDENT[0:64, 0:64], ["SL", "IDENT"], [pk])
                    self.cp("dve", ZS[h2 * 64:(h2 + 1) * 64, g0:g0 + 4, :], ps[0:64, 0:256].rearrange("p (h v) -> p h v", h=4), [pk], ["ZS"])
            step(ZS, "ZS", QS[1][:, :, s:s + 1], QS[0][:, :, s:s + 1], QST[4][:, s, :], QST[2][:, s, :], QST[3][:, s, :],
                 VTS[0:64, :], 64, s, (lambda h2, hs_, hp, s=s: PYSh[h2][hs_, hp, s:s + 1]), s % 2, ["QS", "QST", "VTM"])
            for g0 in range(0, 8, 4):
                u = self.next_unit()
                ps = self.PS[u % 2]
                pk = "PS%d" % (u % 2)
                for hp in range(g0, g0 + 4):
                    self.tr(ps[0:64, (hp - g0) * 128:(hp - g0 + 1) * 128], ZS[:, hp, :], self.IDENT[:, :], ["ZS", "IDENT"], [pk])
                self.cp("act", SOs[:, g0:g0 + 4, :], ps[0:64, 0:512].rearrange("p (h k) -> p h k", h=4), [pk], ["SL"])
            self.dma("sp", self.s_rwkv[i, s0 + s].rearrange("(hp h2) v k -> v hp h2 k", h2=2),
                     SOs[:, :, :].rearrange("p h (a k) -> p h a k", a=2), ["SL"], [], "srw")
        YSs = ytake("YSs", [128, 8, NS], F32, 8 * NS * 4)
        self.cp("act", YSs[0:64, :, :], PYSh[0][0:64, :, :], ["PY0"], ["YST"])
        self.cp("act", YSs[64:128, :, :], PYSh[1][64:128, :, :], ["PY1"], ["YST"])
        self.dma("sp", self.YSD[:, :, TP:TP + NS], YSs[:, :, :], ["YST"], ["YSD"], "ysd")

        postk = ["P1", "P2", "P3", "P4", "P5", "P6", "P7"]
        self.fence(scank + ["PY0", "PY1", "PVB0", "PVB1", "PUB0", "PUB1"], postk)
        yo = self.Y_off
        Ps = [ytake("P%d" % k, [128, NT], F32, NT * 4) for k in range(1, 8)]
        P1, P2, P3, P4, P5, P6, P7_ = Ps
        for hp in range(8):
            self.dma("sp", P1[:, :], self.YSD[:, hp, :], ["YSD"], ["P1"], "pl1")
            self.dma("sp", P2[:, :], self.RWQ[0, :, hp, :], ["RWQ0"], ["P2"], "pl2")
            self.dma("sp", P3[:, :], self.RWQ[3, :, hp, :], ["RWQ3"], ["P3"], "pl3")
            self.dma("sp", P4[:, :], self.RWQ[5, :, hp, :], ["RWQ5"], ["P4"], "pl4")
            self.dma("sp", P5[:, :], self.RWQ[6, :, hp, :], ["RWQ6"], ["P5"], "pl5")
            pp, pk, sb_, sk = self.fp32_cols_mm(self.BONES[:, :], P1, slice(0, 128), ["BONES", "P1"])
            self.stt("dve", P1[:, 0:TP], pp[:, 0:TP], -1.0 / 64, P1[:, 0:TP], ALU.mult, ALU.add, [pk, "P1"], ["P1"])
            self.stt("dve", P1[:, TP:NT], sb_[:, 0:NS], -1.0 / 64, P1[:, TP:NT], ALU.mult, ALU.add, [sk, "P1"], ["P1"])
            self.tt("dve", P6[:, :], P1[:, :], P1[:, :], ALU.mult, ["P1"], ["P6"])
            pp, pk, sb_, sk = self.fp32_cols_mm(self.BONES[:, :], P6, slice(0, 128), ["BONES", "P6"])
            self.act(P6[:, 0:TP], pp[:, 0:TP], AF.Sqrt, [pk], ["P6"], bias=GN_EPS, scale=1.0 / 64)
            self.act(P6[:, TP:NT], sb_[:, 0:NS], AF.Sqrt, [sk], ["P6"], bias=GN_EPS, scale=1.0 / 64)
            self.recip(P6[:, :], P6[:, :], ["P6"], ["P6"])
            self.tt("dve", P1[:, :], P1[:, :], P6[:, :], ALU.mult, ["P1", "P6"], ["P1"])
            self.ts("dve", P1[:, :], P1[:, :], par(5, hp), par(6, hp), ALU.mult, ALU.add, ["P1", "PAR"], ["P1"])
            self.stt("dve", P7_[:, :], P2[:, :], par(4, hp), P3[:, :], ALU.mult, ALU.mult, ["P2", "P3", "PAR"], ["P7"])
            pp, pk, sb_, sk = self.fp32_cols_mm(self.BONES[:, :], P7_, slice(0, 128), ["BONES", "P7"])
            self.tt("dve", P7_[:, 0:TP], P4[:, 0:TP], pp[:, 0:TP], ALU.mult, ["P4", pk], ["P7"])
            self.tt("dve", P7_[:, TP:NT], P4[:, TP:NT], sb_[:, 0:NS], ALU.mult, ["P4", sk], ["P7"])
            self.tt("dve", P1[:, :], P1[:, :], P7_[:, :], ALU.add, ["P1", "P7"], ["P1"])
            self.tt("dve", self.H[:, 8 + hp, :], P1[:, :], P5[:, :], ALU.mult, ["P1", "P5"], ["H%d" % (8 + hp)])
        self.ovk = self.ovk + rwk + scank + postk + ["PY0", "PY1", "PVB0", "PVB1", "PUB0", "PUB1"]

    def rope_q(self, pp, pk, sbank, sk, QT, cq):
        cfg = self.cfg
        TP, NS, NT = cfg.TP, cfg.NS, cfg.NT
        R0 = self.TMP[:, 0, 0:NT]
        R1 = self.TMP[:, 1, 0:NT]
        self.cp("act", R0[:, 0:TP], pp[:, 0:TP], [pk], ["TMP0"])
        self.cp("act", R0[:, TP:NT], sbank[:, 8:8 + NS], [sk], ["TMP0"])
        for (a, b) in ((0, 32), (32, 0), (64, 96), (96, 64)):
            self.cp("dve", R1[a:a + 32, :], R0[b:b + 32, :], ["TMP0"], ["TMP1"])
        self.tt("dve", R0, R0, self.COS[:, :], ALU.mult, ["TMP0", "ROPE"], ["TMP0"])
        self.tt("dve", R1, R1, self.SIN[:, :], ALU.mult, ["TMP1", "ROPE"], ["TMP1"])
        self.tt("dve", QT[:, cq, 0:NT], R0, R1, ALU.add, ["TMP0", "TMP1"], ["QT%d" % cq])

    def swa_prompt(self, i, ps_i, QT, KT, VT, MASK, SSB):
        cfg = self.cfg
        TP, NS, NT = cfg.TP, cfg.NS, cfg.NT
        NB = TP // 128
        ST = self.STAT
        for n in range(NB):
            mi = 1 if (ps_i == 0 and n == 0) else 0
            for kh in range(4):
                u = self.next_unit()
                pp = self.PP[u % 3]
                pk = "PP%d" % (u % 3)
                kkeys = ["KT%d" % kh, "KTc"]
                for g in range(4):
                    h = 4 * kh + g
                    cq, hf = h // 2, h % 2
                    sl_ = hf * 2 + g // 2
                    self.mm(pp[:, sl_ * 256:(sl_ + 1) * 256], QT[hf * 64:(hf + 1) * 64, cq, n * 128:(n + 1) * 128],
                            KT[hf * 64:(hf + 1) * 64, kh, n * 128:n * 128 + 256], True, True, ["QT%d" % cq] + kkeys, [pk])
                for hf in range(2):
                    pvh = pp[:, hf * 512:(hf + 1) * 512].rearrange("p (j k) -> p j k", j=2)
                    sbh = SSB[:, :, :].rearrange("p (j h) k -> p h j k", h=2)[:, hf, :, :]
                    self.stt("dve", sbh, pvh, 0.125, MASK[:, mi:mi + 1, :].to_broadcast([128, 2, 256]), ALU.mult, ALU.add,
                             [pk, "MASK"], ["SSB"])
                self.softmax_rows(SSB, 128, 256, i, kh)
                u2 = self.next_unit()
                pt = self.PP[u2 % 3]
                ptk = "PP%d" % (u2 % 3)
                for g in range(4):
                    for kb in range(2):
                        self.tr(pt[:, (g * 2 + kb) * 128:(g * 2 + kb + 1) * 128], SSB[:, g, kb * 128:(kb + 1) * 128], self.IDENT[:, :],
                                ["SSB", "IDENT"], [ptk])
                PTb = self.WD[:, 0, 0:1024]
                self.cp("dve", PTb, pt[:, :], [ptk], ["WD0"])
                ops_ = self.PS[kh % 2]
                opk = "PS%d" % (kh % 2)
                for g in range(4):
                    h = 4 * kh + g
                    hf = h % 2
                    j = g // 2
                    for kb in range(2):
                        self.mm(ops_[hf * 64:(hf + 1) * 64, j * 128:(j + 1) * 128], VT[:, n + kb, kh * 64:(kh + 1) * 64],
                                PTb[:, (g * 2 + kb) * 128:(g * 2 + kb + 1) * 128], kb == 0, kb == 1,
                                ["VT%d" % (n + kb), "VT0", "WD0"], [opk])
                for j in range(2):
                    cq = 2 * kh + j
                    self.cp("dve", self.H[:, cq, n * 128:(n + 1) * 128], ops_[:, j * 128:(j + 1) * 128], [opk], ["H%d" % cq])

    def softmax_rows(self, SSB, np_, nk, i, kh):
        ST = self.STAT
        sink = self.SINK[0:np_, i * 16 + kh * 4:i * 16 + kh * 4 + 4]
        S = SSB[0:np_, :, 0:nk]
        MX = ST[0:np_, 0:4]
        SM = ST[0:np_, 4:8]
        ES = ST[0:np_, 8:12]
        self.P.op("dve", lambda e: e.tensor_reduce(out=MX, in_=S, axis=AX.X, op=ALU.max), ["SSB"], ["STAT"])
        self.tt("dve", MX, MX, sink, ALU.max, ["STAT", "SINK"], ["STAT"])
        self.tt("dve", S, S, MX.unsqueeze(2).to_broadcast([np_, 4, nk]), ALU.subtract, ["SSB", "STAT"], ["SSB"])
        self.act(S, S, AF.Exp, ["SSB"], ["SSB"])
        self.P.op("dve", lambda e: e.tensor_reduce(out=SM, in_=S, axis=AX.X, op=ALU.add), ["SSB"], ["STAT"])
        self.tt("dve", ES, sink, MX, ALU.subtract, ["STAT", "SINK"], ["STAT"])
        self.act(ES, ES, AF.Exp, ["STAT"], ["STAT"])
        self.tt("dve", SM, SM, ES, ALU.add, ["STAT"], ["STAT"])
        self.recip(SM, SM, ["STAT"], ["STAT"])
        self.tt("dve", S, S, SM.unsqueeze(2).to_broadcast([np_, 4, nk]), ALU.mult, ["SSB", "STAT"], ["SSB"])

    def swa_sample(self, i, ps_i, QT, KT, VCs, KCs, SSB, F4, VSA):
        cfg = self.cfg
        TP, NS, NT = cfg.TP, cfg.NS, cfg.NT
        s0 = ps_i * NS
        W0 = TP
        for s in range(NS):
            rs = s
            col = TP + s
            CK = F4[:, 0, :]
            CV = F4[:, 1, :]
            self.dma("sp", CK, self.cache_k[i, s0 + s], [], ["F4a"], "ckld")
            self.dma("sp", CV, self.cache_v[i, s0 + s], [], ["F4b"], "cvld")
            self.cp("act", VCs[:, :], CV, ["F4b"], ["VCs"])
            for kh in range(4):
                pt = self.PS[kh % 2]
                ptk = "PS%d" % (kh % 2)
                self.tr(pt[0:64, 0:128], CK[:, kh * 64:(kh + 1) * 64], self.IDENT[:, :], ["F4a", "IDENT"], [ptk])
                self.cp("dve", KCs[0:64, kh, :], pt[0:64, 0:128], [ptk], ["KCs"])
                self.cp("dve", KCs[64:128, kh, :], pt[0:64, 0:128], [ptk], ["KCs"])
            for kh in range(4):
                u = self.next_unit()
                pp = self.PP[u % 3]
                pk = "PP%d" % (u % 3)
                for g in range(4):
                    h = 4 * kh + g
                    cq, hf = h // 2, h % 2
                    q = QT[hf * 64:(hf + 1) * 64, cq, W0:W0 + 32]
                    sl_ = hf * 2 + g // 2
                    self.mm(pp[0:32, sl_ * 256:sl_ * 256 + 128], q, KCs[hf * 64:(hf + 1) * 64, kh, :], True, True, ["QT%d" % cq, "KCs"], [pk])
                    self.mm(pp[0:32, sl_ * 256 + 128:sl_ * 256 + 129], q, KT[hf * 64:(hf + 1) * 64, kh, 128 + col:128 + col + 1], False, True,
                            ["QT%d" % cq, "KT%d" % kh], [pk], skip_group_check=True)
                for hf in range(2):
                    pvh = pp[0:32, hf * 512:(hf + 1) * 512].rearrange("p (j k) -> p j k", j=2)
                    sbh = SSB[0:32, :, :].rearrange("p (j h) k -> p h j k", h=2)[:, hf, :, :]
                    self.ts("dve", sbh[:, :, 0:129], pvh[:, :, 0:129], 0.125, None, ALU.mult, None, [pk], ["SSB"])
                self.softmax_rows(SSB, 32, 129, i, kh)
                pt = self.PS[kh % 2]
                ptk = "PS%d" % (kh % 2)
                for g in range(4):
                    self.tr(pt[:, g * 32:(g + 1) * 32], SSB[0:32, g, 0:128], self.IDENT[0:32, 0:32], ["SSB", "IDENT"], [ptk])
                PT4 = self.ACTB[:, 1, 0:4]
                self.cp("dve", PT4, pt[:, 0:128].rearrange("p (g r) -> p g r", r=32)[:, :, rs], [ptk], ["ACTB1"])
                self.memset("dve", self.STAT[:, 20:24], 0.0, ["STAT"])
                self.cp("dve", self.STAT[0:32, 16:20], SSB[0:32, :, 128], ["SSB"], ["STAT"])
                self.ts("dve", self.STAT[0:32, 20:24], self.STAT[0:32, 16:20], self.IDENT[0:32, rs:rs + 1], None, ALU.mult, None,
                        ["STAT", "IDENT"], ["STAT"])
                for g in range(4):
                    h = 4 * kh + g
                    hf = h % 2
                    j = g // 2
                    o_ = pt[hf * 64:(hf + 1) * 64, 256 + j:257 + j]
                    self.mm(o_, VCs[:, kh * 64:(kh + 1) * 64], PT4[:, g:g + 1], True, True, ["VCs", "ACTB1"], [ptk], skip_group_check=True)
                    o2 = pt[hf * 64:(hf + 1) * 64, 264 + j:265 + j]
                    self.mm(o2, VSA[:, kh * 64:(kh + 1) * 64], self.STAT[:, 20 + g:21 + g], True, True, ["VSA", "STAT"], [ptk],
                            skip_group_check=True)
                for j in range(2):
                    cq = 2 * kh + j
                    self.cp("dve", self.STAT[:, 24:25], pt[:, 256 + j:257 + j], [ptk], ["STAT"])
                    self.tt("dve", self.H[:, cq, col:col + 1], self.STAT[:, 24:25], pt[:, 264 + j:265 + j], ALU.add, [ptk, "STAT"], ["H%d" % cq])


_NC_CACHE = {}


def get_program(cfg_key):
    if cfg_key not in _NC_CACHE:
        cfg = Cfg(*cfg_key)
        b = Builder(cfg)
        b.build()
        _NC_CACHE[cfg_key] = b
    return _NC_CACHE[cfg_key]


def rope_tables(cfg):
    half = 32
    freqs = (np.float32(10000.0) ** (-np.arange(half, dtype=np.float32) / np.float32(half))).astype(np.float32)
    out = np.zeros((cfg.NPASS, 2, 128, cfg.NT), np.float32)
    for ps_i in range(cfg.NPASS):
        pos = np.concatenate([ps_i * cfg.TP + np.arange(cfg.TP), np.full(cfg.NS, 16384)]).astype(np.float32)
        ang = (pos[None, :] * freqs[:, None]).astype(np.float32)
        c = np.cos(ang).astype(np.float32)
        sn = np.sin(ang).astype(np.float32)
        cosF = np.concatenate([c, c, c, c], 0)
        sinS = np.concatenate([-sn, sn, -sn, sn], 0)
        out[ps_i, 0] = cosF
        out[ps_i, 1] = sinS
    return out


def swa_masks():
    qi = np.arange(128)[:, None]
    kc = np.arange(256)[None, :]
    diff = 128 + qi - kc
    band = (diff >= 0) & (diff <= 128)
    m0 = np.where(band, 0.0, -1e30).astype(np.float32)
    m1 = np.where(band & (kc >= 128), 0.0, -1e30).astype(np.float32)
    return np.stack([m0, m1], 0)


def run(inputs, cfg_key, n_cores=8):
    cfg = Cfg(*cfg_key)
    b = get_program(cfg_key)
    TP, NS, NPASS, L = cfg.TP, cfg.NS, cfg.NPASS, cfg.DEPTH
    NAB, NCV = cfg.N_AB, cfg.N_CV
    f32 = lambda a: np.ascontiguousarray(a, dtype=np.float32)
    x_prompt = np.asarray(inputs["x_prompt"], np.float32)
    x_sample = np.asarray(inputs["x_sample"], np.float32)
    nb = x_prompt.shape[0]
    spc = NPASS * NS
    nsc = max(1, x_sample.shape[0] // spc)
    shared = {
        "norm_g": f32(inputs["norm_g"][:L]),
        "ffn_w_gu": f32(inputs["ffn_w_gu"][:L]),
        "ffn_w_down": f32(inputs["ffn_w_down"][:L]),
    }
    if cfg.mixers:
        shared.update({
            "conv_w_in": f32(inputs["conv_w_in"][:NCV]), "conv_w": f32(inputs["conv_w"][:NCV]),
            "conv_w_out": f32(inputs["conv_w_out"][:NCV]),
            "ab_w_in": f32(inputs["ab_w_in"][:NAB]), "ab_w_out": f32(inputs["ab_w_out"][:NAB]),
            "attn_sinks": f32(inputs["attn_sinks"][:NAB]),
            "rope_t": rope_tables(cfg), "swa_masks": swa_masks(),
            "rwkv_par": f32(np.stack([inputs[k][:NAB] for k in ("rwkv_w0", "rwkv_a0", "rwkv_k_k", "rwkv_k_a", "rwkv_r_k",
                                                                "rwkv_gn_w", "rwkv_gn_b")], 1)),
            "rwkv_mu": f32(inputs["rwkv_mu"][:NAB]), "rwkv_w_decay": f32(inputs["rwkv_w_decay"][:NAB]),
            "rwkv_w_aaa": f32(inputs["rwkv_w_aaa"][:NAB]), "rwkv_w_gate": f32(inputs["rwkv_w_gate"][:NAB]),
        })
    in_maps = []
    for c in range(n_cores):
        bb = c % nb
        sc = c % nsc
        sl = slice(sc * spc, (sc + 1) * spc)
        m = dict(shared)
        m["xp"] = f32(x_prompt[bb])
        m["xs"] = f32(x_sample[sl, 0, :])
        if cfg.mixers:
            m["state_conv"] = f32(inputs["state_conv"][:NCV, sl])
            m["cache_swa_k"] = f32(inputs["cache_swa_k"][:NAB, sl].reshape(NAB, spc, 128, 256))
            m["cache_swa_v"] = f32(inputs["cache_swa_v"][:NAB, sl].reshape(NAB, spc, 128, 256))
            m["state_rwkv"] = f32(inputs["state_rwkv"][:NAB, sl])
            m["state_rwkv_shift"] = f32(inputs["state_rwkv_shift"][:NAB, sl])
        m = {k: v for k, v in m.items() if k in b.din}
        in_maps.append(m)
    res = run_bass_kernel_spmd(b.nc, in_maps, core_ids=list(range(n_cores)))
    rs = res.results
    pc = list(range(min(nb, n_cores)))
    sc_ = list(range(min(nsc, n_cores)))
    out = {}
    out["y_prompt"] = np.stack([rs[c]["y_prompt"] for c in pc], 0)
    out["y_sample"] = np.concatenate([rs[c]["y_sample"] for c in sc_], 0)[:, None, :]
    if cfg.mixers:
        def pst(name, shp):
            return np.stack([rs[c][name] for c in pc], 1).reshape(shp) if name in rs[0] else None
        def sst(name, shp):
            return np.concatenate([rs[c][name] for c in sc_], 1).reshape(shp) if name in rs[0] else None
        nbp, nss = len(pc), len(sc_) * spc
        out["p_swa_k"] = pst("p_swa_k", (NAB, nbp, 128, 4, 64))
        out["p_swa_v"] = pst("p_swa_v", (NAB, nbp, 128, 4, 64))
        out["p_rwkv"] = pst("p_rwkv", (NAB, nbp, 16, 64, 64))
        out["p_shift"] = pst("p_shift", (NAB, nbp, B_COLS))
        out["p_conv"] = pst("p_conv", (NCV, nbp, 2, D))
        out["s_swa_k"] = sst("s_swa_k", (NAB, nss, 128, 4, 64))
        out["s_swa_v"] = sst("s_swa_v", (NAB, nss, 128, 4, 64))
        out["s_rwkv"] = sst("s_rwkv", (NAB, nss, 16, 64, 64))
        out["s_shift"] = sst("s_shift", (NAB, nss, B_COLS))
        out["s_conv"] = sst("s_conv", (NCV, nss, 2, D))
    return out


FULL_CFG = (1024, 4, 2, 4, 3)


def kernel(**inputs):
    out = run(inputs, FULL_CFG, n_cores=8)
    names = ["y_prompt", "y_sample", "p_swa_k", "p_swa_v", "p_rwkv", "p_shift", "p_conv",
             "s_swa_k", "s_swa_v", "s_rwkv", "s_shift", "s_conv"]
    return tuple(np.ascontiguousarray(out[n], dtype=np.float32) for n in names)
```
